# Optimizing a Trainium2 kernel written in Bass

```python
import jax, jax.numpy as jnp
from jax import lax
import numpy as np

D_MODEL = 1024
BATCH = 4
SEQ = 8192
DEPTH = 4

N_CONV_LAYERS = DEPTH // 2
N_ATTN_LAYERS = DEPTH - N_CONV_LAYERS
CONV_WIDTH = 3
N_HEADS = 16
HEAD_DIM = D_MODEL // N_HEADS
Q_BLOCK = 128
N_EXPERTS = 32
TOP_K = 4
D_EXPERT = D_MODEL
SWIGLU_LIMIT = 7.0
SWIGLU_ALPHA = 1.702
EXPERT_BLOCK = 128
DEEPNORM_ALPHA = (2.0 * DEPTH) ** 0.25
DEEPNORM_BETA = (8.0 * DEPTH) ** -0.25
LN_EPS = 1e-5
ADA_SCALE = 0.1
FORGET_BIAS_INIT = 3.0

kernel_name = "yoco_shortconv_fox_moe_deepnorm"


def layer_norm(x, g, b):
    xf = x.astype(jnp.float32)
    mu = jnp.mean(xf, axis=-1, keepdims=True)
    var = jnp.mean(jnp.square(xf - mu), axis=-1, keepdims=True)
    y = (xf - mu) * lax.rsqrt(var + LN_EPS) * g.astype(jnp.float32) + b.astype(jnp.float32)
    return y.astype(x.dtype)


def ada_params(cond, w, b):
    return jnp.split(cond @ w + b, 3, axis=-1)


def modulate(x, shift, scale):
    return x * (1 + scale[:, None, :]) + shift[:, None, :]


def deepnorm_residual(x, sub, gate, g, b):
    return layer_norm(DEEPNORM_ALPHA * x + (1 + gate[:, None, :]) * sub, g, b)


def short_conv_mixer(h, w_in, w_conv, w_out):
    gate_c, gate_b, u = jnp.split(h @ w_in, 3, axis=-1)
    z = gate_c * u
    z = lax.conv_general_dilated(
        z, w_conv[:, None, :].astype(z.dtype), window_strides=(1,),
        padding=[(CONV_WIDTH - 1, 0)], dimension_numbers=("NWC", "WIO", "NWC"),
        feature_group_count=D_MODEL)
    return (gate_b * z) @ w_out


def shared_kv(x, cond, kv_ada_w, kv_ada_b, w_kvf, b_f):
    bsz, seq, _ = x.shape
    shift, scale = jnp.split(cond @ kv_ada_w + kv_ada_b, 2, axis=-1)
    kvf = modulate(x, shift, scale) @ w_kvf
    k = kvf[..., :D_MODEL].reshape(bsz, seq, N_HEADS, HEAD_DIM).transpose(0, 2, 1, 3)
    v = kvf[..., D_MODEL:2 * D_MODEL].reshape(bsz, seq, N_HEADS, HEAD_DIM).transpose(0, 2, 1, 3)
    log_f = jax.nn.log_sigmoid(kvf[..., 2 * D_MODEL:].astype(jnp.float32) + b_f.astype(jnp.float32))
    log_f_cum = jnp.cumsum(log_f, axis=1).transpose(0, 2, 1)
    return k, v, log_f_cum


def forgetting_attention(h, w_q, w_o, k, v, log_f_cum):
    bsz, seq, _ = h.shape
    nb = seq // Q_BLOCK
    q = (h @ w_q).reshape(bsz, nb, Q_BLOCK, N_HEADS, HEAD_DIM).transpose(1, 0, 3, 2, 4)
    dq = log_f_cum.reshape(bsz, N_HEADS, nb, Q_BLOCK).transpose(2, 0, 1, 3)
    k_pos = jnp.arange(seq)
    scale = HEAD_DIM ** -0.5

    def block(args):
        qb, dqb, i = args
        q_pos = i * Q_BLOCK + jnp.arange(Q_BLOCK)
        s = jnp.einsum('bhqd,bhkd->bhqk', qb, k, preferred_element_type=jnp.float32) * scale
        s = s + (dqb[..., :, None] - log_f_cum[..., None, :])
        s = jnp.where(q_pos[:, None] >= k_pos[None, :], s, -jnp.inf)
        p = jax.nn.softmax(s, axis=-1)
        return jnp.einsum('bhqk,bhkd->bhqd', p.astype(v.dtype), v)

    o = lax.map(block, (q, dq, jnp.arange(nb)))
    o = o.transpose(1, 0, 3, 2, 4).reshape(bsz, seq, D_MODEL)
    return o @ w_o


def routed_moe(h, w_r, b_r, w_gu, b_gu, w_down, b_down):
    n_tok = h.shape[0]
    n_assign = n_tok * TOP_K
    logits = jnp.dot(h, w_r, preferred_element_type=jnp.float32) + b_r.astype(jnp.float32)
    top_val, top_idx = lax.top_k(logits, TOP_K)
    top_w = jax.nn.softmax(top_val, axis=-1)
    flat_e = top_idx.reshape(-1)
    flat_tok = jnp.arange(n_assign, dtype=jnp.int32) // TOP_K
    order = jnp.argsort(flat_e, stable=True)
    sorted_e = flat_e[order]
    counts = jnp.bincount(flat_e, length=N_EXPERTS)
    padded = (counts + EXPERT_BLOCK - 1) // EXPERT_BLOCK * EXPERT_BLOCK
    padded_end = jnp.cumsum(padded)
    padded_start = padded_end - padded
    group_start = jnp.cumsum(counts) - counts
    dest = padded_start[sorted_e] + jnp.arange(n_assign, dtype=jnp.int32) - group_start[sorted_e]
    n_blocks = -(-n_assign // EXPERT_BLOCK) + N_EXPERTS
    n_slots = n_blocks * EXPERT_BLOCK
    slot_tok = jnp.full((n_slots,), n_tok, jnp.int32).at[dest].set(flat_tok[order])
    slot_w = jnp.zeros((n_slots,), jnp.float32).at[dest].set(top_w.reshape(-1)[order])
    block_expert = jnp.minimum(
        jnp.searchsorted(padded_end, jnp.arange(n_blocks) * EXPERT_BLOCK, side='right'), N_EXPERTS - 1)
    h_pad = jnp.concatenate([h, jnp.zeros((1, h.shape[1]), h.dtype)], axis=0)

    def expert_block(args):
        tok, e, wt = args
        gu = h_pad[tok] @ w_gu[e] + b_gu[e]
        g = jnp.minimum(gu[:, :D_EXPERT], SWIGLU_LIMIT)
        u = jnp.clip(gu[:, D_EXPERT:], -SWIGLU_LIMIT, SWIGLU_LIMIT)
        a = g * jax.nn.sigmoid(SWIGLU_ALPHA * g) * (u + 1)
        y = a @ w_down[e] + b_down[e]
        return y * wt[:, None].astype(y.dtype)

    y = lax.map(expert_block, (slot_tok.reshape(n_blocks, EXPERT_BLOCK), block_expert,
                               slot_w.reshape(n_blocks, EXPERT_BLOCK)))
    return jax.ops.segment_sum(y.reshape(n_slots, -1), slot_tok, num_segments=n_tok + 1)[:n_tok]


def setup_inputs(seed: int = 0) -> dict:
    key = jax.random.key(seed)
    ks = jax.random.split(key, 24)
    D, H, E, F = D_MODEL, N_HEADS, N_EXPERTS, D_EXPERT
    nrm = lambda k, shape, s: jax.random.normal(k, shape, jnp.float32) * s
    w_kvf = jnp.concatenate([
        nrm(ks[6], (D, D), D ** -0.5),
        nrm(ks[7], (D, D), D ** -0.5 * DEEPNORM_BETA),
        nrm(ks[8], (D, H), D ** -0.5)], axis=1)
    return {
        "x": nrm(ks[0], (BATCH, SEQ, D), 1.0),
        "c": nrm(ks[1], (BATCH, D), 1.0),
        "conv_w_in": nrm(ks[2], (N_CONV_LAYERS, D, 3 * D), D ** -0.5),
        "conv_w": nrm(ks[3], (N_CONV_LAYERS, CONV_WIDTH, D), CONV_WIDTH ** -0.5),
        "conv_w_out": nrm(ks[4], (N_CONV_LAYERS, D, D), D ** -0.5 * DEEPNORM_BETA),
        "kv_ada_w": nrm(ks[5], (D, 2 * D), ADA_SCALE * D ** -0.5),
        "kv_ada_b": nrm(ks[9], (2 * D,), 0.01),
        "w_kvf": w_kvf,
        "b_f": FORGET_BIAS_INIT + nrm(ks[10], (H,), 0.1),
        "attn_w_q": nrm(ks[11], (N_ATTN_LAYERS, D, D), D ** -0.5),
        "attn_w_o": nrm(ks[12], (N_ATTN_LAYERS, D, D), D ** -0.5 * DEEPNORM_BETA),
        "ada_w": nrm(ks[13], (DEPTH, 2, D, 3 * D), ADA_SCALE * D ** -0.5),
        "ada_b": nrm(ks[14], (DEPTH, 2, 3 * D), 0.01),
        "ln_g": 1.0 + nrm(ks[15], (DEPTH, 2, D), 0.02),
        "ln_b": nrm(ks[16], (DEPTH, 2, D), 0.02),
        "router_w": nrm(ks[17], (DEPTH, D, E), D ** -0.5),
        "router_b": nrm(ks[18], (DEPTH, E), 0.01),
        "exp_w_gu": nrm(ks[19], (DEPTH, E, D, 2 * F), D ** -0.5),
        "exp_b_gu": nrm(ks[20], (DEPTH, E, 2 * F), 0.01),
        "exp_w_down": nrm(ks[21], (DEPTH, E, F, D), F ** -0.5 * DEEPNORM_BETA),
        "exp_b_down": nrm(ks[22], (DEPTH, E, D), 0.01),
    }


def reference(x, c, conv_w_in, conv_w, conv_w_out, kv_ada_w, kv_ada_b, w_kvf, b_f,
              attn_w_q, attn_w_o, ada_w, ada_b, ln_g, ln_b, router_w, router_b,
              exp_w_gu, exp_b_gu, exp_w_down, exp_b_down):
    cond = jax.nn.silu(c)
    k = v = log_f_cum = None
    for l in range(DEPTH):
        shift, scale, gate = ada_params(cond, ada_w[l, 0], ada_b[l, 0])
        h = modulate(x, shift, scale)
        if l < N_CONV_LAYERS:
            sub = short_conv_mixer(h, conv_w_in[l], conv_w[l], conv_w_out[l])
        else:
            j = l - N_CONV_LAYERS
            sub = forgetting_attention(h, attn_w_q[j], attn_w_o[j], k, v, log_f_cum)
        x = deepnorm_residual(x, sub, gate, ln_g[l, 0], ln_b[l, 0])
        shift, scale, gate = ada_params(cond, ada_w[l, 1], ada_b[l, 1])
        h = modulate(x, shift, scale)
        sub = routed_moe(h.reshape(-1, D_MODEL), router_w[l], router_b[l], exp_w_gu[l],
                         exp_b_gu[l], exp_w_down[l], exp_b_down[l]).reshape(x.shape)
        x = deepnorm_residual(x, sub, gate, ln_g[l, 1], ln_b[l, 1])
        if l == N_CONV_LAYERS - 1:
            k, v, log_f_cum = shared_kv(x, cond, kv_ada_w, kv_ada_b, w_kvf, b_f)
    return x
```

```python
import contextlib
import numpy as np
import ml_dtypes
import concourse.bass as bass
import concourse.mybir as mybir
from concourse.bass_utils import run_bass_kernel_spmd

F32 = mybir.dt.float32
BF16 = mybir.dt.bfloat16
I32 = mybir.dt.int32
ALU = mybir.AluOpType
AF = mybir.ActivationFunctionType
AX = mybir.AxisListType

D = 1024
NCH = 8
H = 16
DH = 64
TOPK = 4
ALPHA = (2.0 * 4) ** 0.25
LN_EPS = 1e-5
LIMIT = 7.0
SW_ALPHA = 1.702
SCALE = DH ** -0.5
ROWW = 1024 + 64

SELF_WAIT = True
ENGS = ["pe", "act", "dve", "pool", "sp"]
BLK = {"pe": "tensor", "act": "scalar", "dve": "vector", "pool": "gpsimd", "sp": "sync"}


class Buf:
    __slots__ = ("w", "r", "sem", "cnt", "name")

    def __init__(self, name=""):
        self.w = {}
        self.r = {}
        self.sem = None
        self.cnt = 0
        self.name = name


class Prog:
    def __init__(self, nc, stack):
        self.nc = nc
        self.stack = stack
        self.sems = {}
        self.ecnt = {}
        for e in ["pe", "act", "dve", "pool"]:
            self.sems[("e", e)] = stack.enter_context(nc.semaphore("s_" + e))
            self.ecnt[e] = 0
        self.ndma = 0
        self.dma_bufs = []
        self.free_sems = []
        self.q = {e: [] for e in ENGS}
        self.waited = {e: {} for e in ENGS}
        self.ninstr = 0

    def _deps(self, e, r, w, acc):
        deps = {}
        for b in r:
            for k, v in b.w.items():
                if deps.get(k, 0) < v:
                    deps[k] = v
        for b in w:
            for dd in (b.w, b.r):
                for k, v in dd.items():
                    if deps.get(k, 0) < v:
                        deps[k] = v
        for b in acc:
            for k, v in b.r.items():
                if deps.get(k, 0) < v:
                    deps[k] = v
        wd = self.waited[e]
        for k, v in deps.items():
            if k == ("e", e) and (e == "pe" or (not SELF_WAIT and e in ("act", "dve"))):
                continue
            if wd.get(k, 0) < v:
                wd[k] = v
                sem = self.sems[k]
                self.q[e].append(lambda eng, sem=sem, v=v: eng.wait_ge(sem, v))
                self.ninstr += 1

    def _mark(self, key, val, r, w, acc):
        for b in r:
            if b.r.get(key, 0) < val:
                b.r[key] = val
        for b in w:
            b.w = {key: val}
            b.r = {}
        for b in acc:
            if b.w.get(key, 0) < val:
                b.w[key] = val

    def op(self, e, fn, r=(), w=(), acc=()):
        self._deps(e, r, w, acc)
        self.ecnt[e] += 1
        key = ("e", e)
        sem = self.sems[key]
        self.q[e].append(lambda eng, fn=fn, sem=sem: fn(eng).then_inc(sem, 1))
        self.ninstr += 1
        self._mark(key, self.ecnt[e], r, w, acc)

    def dma(self, e, fn, sb, r=(), w=(), acc=()):
        self._deps(e, r, w, acc)
        if sb.sem is None:
            if self.free_sems:
                sb.sem, sb.cnt = self.free_sems.pop()
            else:
                sb.sem = ("d", self.ndma)
                sb.cnt = 0
                self.sems[sb.sem] = self.stack.enter_context(self.nc.semaphore("d%d" % self.ndma))
                self.ndma += 1
            self.dma_bufs.append(sb)
        sb.cnt += 1
        key = sb.sem
        sem = self.sems[key]
        self.q[e].append(lambda eng, fn=fn, sem=sem: fn(eng).then_inc(sem, 16))
        self.ninstr += 1
        self._mark(key, 16 * sb.cnt, r, w, acc)

    def flush(self):
        wd = self.waited["sp"]
        for sb in self.dma_bufs:
            if sb.cnt and wd.get(sb.sem, 0) < 16 * sb.cnt:
                wd[sb.sem] = 16 * sb.cnt
                sem = self.sems[sb.sem]
                self.q["sp"].append(lambda eng, sem=sem, v=16 * sb.cnt: eng.wait_ge(sem, v))
        with self.nc.Block() as block:
            for e in ENGS:
                if self.q[e]:
                    L = self.q[e]

                    def body(eng, L=L):
                        for f in L:
                            f(eng)

                    getattr(block, BLK[e])(body)
        self.q = {e: [] for e in ENGS}
        full = {}
        for e in ["pe", "act", "dve", "pool"]:
            full[("e", e)] = self.ecnt[e]
        for sb in self.dma_bufs:
            full[sb.sem] = 16 * sb.cnt
        for e in ENGS:
            self.waited[e].update(full)
        for sb in self.dma_bufs:
            self.free_sems.append((sb.sem, sb.cnt))
            sb.sem = None
        self.dma_bufs = []


class Cfg:
    def __init__(self, T1, E, cap_mult=1.375):
        self.T1 = T1
        self.T2 = T1 // 2
        self.E = E
        self.NT1 = T1 // 128
        self.NT2 = self.T2 // 128

        def cap(T):
            mean = T * TOPK / E
            c = int(np.ceil(mean * cap_mult / 128.0)) * 128
            return max(c, 128)

        self.C1 = cap(self.T1)
        cap_mult = max(cap_mult, 1.5)
        self.C2 = cap(self.T2)
        self.upto = 3


def build_program(cfg, debug=False):
    T1, T2, E = cfg.T1, cfg.T2, cfg.E
    NT1, NT2 = cfg.NT1, cfg.NT2
    nc = bass.Bass("TRN2", target_bir_lowering=False)

    def din(name, shape, dt=F32):
        return nc.dram_tensor(name, list(shape), dt, kind="ExternalInput")

    x_in = din("x", [T1, D])
    cT_in = din("cT", [128, NCH])
    conv_w_in = din("conv_w_in", [2, D, 3 * D])
    conv_wT = din("conv_wT", [128, 2, NCH, 3])
    conv_w_out = din("conv_w_out", [2, D, D])
    kv_ada_w = din("kv_ada_w", [D, 2 * D])
    kv_ada_bT = din("kv_ada_bT", [128, 16])
    w_kvf = din("w_kvf", [D, 2 * D + H])
    b_f_bc = din("b_f_bc", [128, H])
    attn_w_q = din("attn_w_q", [2, D, D])
    attn_w_o = din("attn_w_o", [2, D, D])
    ada_w = din("ada_w", [8, D, 3 * D])
    ada_bT = din("ada_bT", [128, 8, 24])
    ln_g_bc = din("ln_g_bc", [8, 128, D])
    ln_b_bc = din("ln_b_bc", [8, 128, D])
    router_w = din("router_w", [4, D, E])
    router_b_bc = din("router_b_bc", [4, 128, E])
    exp_w_gu = din("exp_w_gu", [4, E, D, 2 * D])
    exp_b_guT = din("exp_b_guT", [4, 128, E, 16])
    exp_w_down = din("exp_w_down", [4, E, D, D])
    exp_b_down = din("exp_b_down", [4, E, D])
    ident_f_in = din("ident_f", [128, 128])
    ident_b_in = din("ident_b", [128, 128], BF16)
    ustrict_in = din("ustrict", [128, 128])
    uincl_in = din("uincl", [128, 128])
    ones_in = din("ones_f", [128, 128])
    maskb_in = din("maskb", [128, 128], BF16)
    ebase_in = din("ebase", [128, 4, E])
    selh_in = din("selh", [16, H, 128])
    sel64_in = din("sel64", [128, 64])
    idx_in = din("idxs", [128, 4], I32)
    idx16_in = din("idx16", [128, 1], I32)

    out_t = nc.dram_tensor("out", [T2, D], F32, kind="ExternalOutput")

    def dscr(name, shape, dt=F32):
        if debug and name in debug:
            return nc.dram_tensor(name, list(shape), dt, kind="ExternalOutput")
        return nc.dram_tensor(name, list(shape), dt)

    XA = dscr("XA", [T1, D])
    XB = dscr("XB", [T1, D])
    NG1 = NT1 // 4
    NGB = NT2 // 4
    X1B = [nc.dram_tensor("X1B%d" % g, [2 * 128, 4 * D], F32) for g in range(NGB)]
    XS = dscr("XS", [E * cfg.C1, ROWW], BF16)
    YS = dscr("YS", [E * cfg.C1, D], F32)
    SIDX = dscr("SIDX", [128, NT1 * 4], I32)
    KT = [nc.dram_tensor("KT%d" % h, [3 * 64, T2], BF16) for h in range(H)]
    VA = [nc.dram_tensor("VA%d" % h, [3 * 128, NT2 * 128], BF16) for h in range(H)]
    LF = nc.dram_tensor("LF", [3 * 128, NT2 * H], F32)
    LFT = nc.dram_tensor("LFT", [3 * 16, T2], F32)
    QT = nc.dram_tensor("QT", [H, 128, T2], BF16)
    OT = nc.dram_tensor("OT", [H, 64, T2], BF16)
    dbg = {}

    stack = contextlib.ExitStack()
    with stack:
        P = Prog(nc, stack)

        uniq = [0]

        def sb(name, shape, dt=F32, st=stack):
            uniq[0] += 1
            return st.enter_context(nc.sbuf_tensor("%s_s%d" % (name, uniq[0]), list(shape), dt))

        def ps(name, shape, dt=F32, st=stack):
            uniq[0] += 1
            return st.enter_context(nc.psum_tensor("%s_p%d" % (name, uniq[0]), list(shape), dt))

        ident_f = sb("ident_f", [128, 128])
        ident_b = sb("ident_b", [128, 128], BF16)
        ustrict = sb("ustrict", [128, 128])
        uincl = sb("uincl", [128, 128])
        ones_f = sb("ones_f", [128, 128])
        maskb = sb("maskb", [128, 128], BF16)
        ebase = sb("ebase", [128, 4, E])
        selh = sb("selh", [16, H, 128])
        sel64 = sb("sel64", [128, 64])
        idxs = sb("idxs", [128, 4], I32)
        idx16 = sb("idx16", [128, 1], I32)
        ada = sb("ada", [128, 8, 24])
        kvada = sb("kvada", [128, 16])
        convw = sb("convw", [128, 2, NCH, 3])
        bfb = sb("bfb", [128, H])
        sidx = sb("sidx", [128, NT1, 4], I32)
        cst = Buf("cst")

        for (t, src) in [(ident_f, ident_f_in), (ident_b, ident_b_in), (ustrict, ustrict_in), (uincl, uincl_in),
                         (ones_f, ones_in), (maskb, maskb_in), (ebase, ebase_in), (selh, selh_in),
                         (sel64, sel64_in), (idxs, idx_in), (idx16, idx16_in), (convw, conv_wT), (bfb, b_f_bc)]:
            P.dma("sp", lambda eng, t=t, src=src: eng.dma_start(out=t[:], in_=src.ap()), cst, w=[cst])

        with contextlib.ExitStack() as st:
            cT = sb("cT", [128, NCH], st=st)
            cond = sb("cond", [128, NCH], st=st)
            adab = sb("adab", [128, 8, 24], st=st)
            kvb = sb("kvb", [128, 16], st=st)
            wt = [sb("adaw%d" % i, [128, NCH, 512], st=st) for i in range(2)]
            wtb = [Buf() for _ in range(2)]
            pacc = ps("pacc", [128, 512], st=st)
            b_c, b_cond, b_ps, b_ada = Buf(), Buf(), Buf(), Buf()
            P.dma("sp", lambda eng: eng.dma_start(out=cT[:], in_=cT_in.ap()), b_c, w=[b_c])
            P.dma("sp", lambda eng: eng.dma_start(out=adab[:], in_=ada_bT.ap()), b_c, w=[b_c])
            P.dma("sp", lambda eng: eng.dma_start(out=kvb[:], in_=kv_ada_bT.ap()), b_c, w=[b_c])
            P.op("act", lambda eng: eng.activation(out=cond[:], in_=cT[:], func=AF.Silu), r=[b_c], w=[b_cond])
            it = 0
            jobs = [(lj, ada_w.ap()[lj], 6, lj) for lj in range(8)] + [(8, kv_ada_w.ap(), 4, None)]
            for (lj, wsrc, ncb, _) in jobs:
                for cb in range(ncb):
                    i = it % 2
                    it += 1
                    src = wsrc[:, cb * 512:(cb + 1) * 512].rearrange("(k p) n -> p k n", p=128)
                    P.dma("sp", lambda eng, i=i, src=src: eng.dma_start(out=wt[i][:], in_=src), wtb[i], w=[wtb[i]])
                    for m in range(4):
                        col = cb * 4 + m
                        for k in range(NCH):
                            P.op("pe", lambda eng, i=i, m=m, k=k, col=col: eng.matmul(
                                pacc[:, col:col + 1], lhsT=wt[i][:, k, m * 128:(m + 1) * 128], rhs=cond[:, k:k + 1],
                                start=(k == 0), stop=(k == NCH - 1)), r=[wtb[i], b_cond], w=[b_ps])
                ncol = ncb * 4
                if lj < 8:
                    P.op("dve", lambda eng, lj=lj: eng.tensor_tensor(out=ada[:, lj, :], in0=pacc[:, 0:24], in1=adab[:, lj, :],
                                                                  op=ALU.add), r=[b_ps, b_c], w=[b_ada])
                    P.op("dve", lambda eng, lj=lj: eng.tensor_scalar(out=ada[:, lj, 8:24], in0=ada[:, lj, 8:24], scalar1=1.0,
                                                                  scalar2=None, op0=ALU.add), r=[b_ada], w=[b_ada])
                else:
                    P.op("dve", lambda eng: eng.tensor_tensor(out=kvada[:], in0=pacc[:, 0:16], in1=kvb[:], op=ALU.add),
                         r=[b_ps, b_c], w=[b_ada])
                    P.op("dve", lambda eng: eng.tensor_scalar(out=kvada[:, 8:16], in0=kvada[:, 8:16], scalar1=1.0,
                                                           scalar2=None, op0=ALU.add), r=[b_ada], w=[b_ada])
            P.flush()

        def bc_from_cols(st, name, cols_ap_fn, pbank, out_tile, deps_b):
            diag = sb(name + "_dg", [128, 128], st=st)
            bd, bp = Buf(), Buf()
            for k in range(NCH):
                P.op("dve", lambda eng, k=k: eng.tensor_scalar(out=diag[:], in0=ident_f[:], scalar1=cols_ap_fn(k),
                                                             scalar2=None, op0=ALU.mult), r=[cst], w=[bd])
                P.op("pe", lambda eng: eng.matmul(pbank[:, 0:128], lhsT=ones_f[:], rhs=diag[:], start=True, stop=True),
                     r=[bd, cst], w=[bp])
                P.op("act", lambda eng, k=k: eng.activation(out=out_tile[:, k * 128:(k + 1) * 128], in_=pbank[:, 0:128],
                                                          func=AF.Copy), r=[bp], w=[deps_b])

        def load_w_bf16(tile3, src2d, b, eng_name="pool"):
            src = src2d.rearrange("(k p) n -> p k n", p=128)
            P.dma(eng_name, lambda eng: eng.dma_start(out=tile3, in_=src), b, w=[b])

        def residual_ln(xt, sub_ps_list, g1bc, lngbc, lnbbc, t0, t1, outt, b_x, b_sub, b_bc, b_t0, b_t1, b_out, st_small):
            stats, mv, rstd = st_small
            for (pap, c0, n) in sub_ps_list:
                P.op("dve", lambda eng, pap=pap, c0=c0, n=n: eng.tensor_tensor(out=t0[:, c0:c0 + n], in0=pap, in1=g1bc[:, c0:c0 + n],
                                                                             op=ALU.mult), r=[b_sub, b_bc], w=[b_t0])
            P.op("dve", lambda eng: eng.scalar_tensor_tensor(out=t1[:], in0=xt, scalar=ALPHA, in1=t0[:], op0=ALU.mult,
                                                           op1=ALU.add), r=[b_x, b_t0], w=[b_t1])
            for c in range(2):
                P.op("dve", lambda eng, c=c: eng.bn_stats(out=stats[:, c, :], in_=t1[:, c * 512:(c + 1) * 512]), r=[b_t1], w=[b_t0])
            P.op("dve", lambda eng: eng.bn_aggr(out=mv[:], in_=stats[:].rearrange("p a b -> p (a b)")), r=[b_t0], w=[b_t0])
            P.op("act", lambda eng: eng.activation(out=rstd[:], in_=mv[:, 1:2], func=AF.Sqrt, bias=epsc[:, 0:1], scale=1.0),
                 r=[b_t0, cst], w=[b_t0])
            P.op("dve", lambda eng: eng.reciprocal(out=rstd[:], in_=rstd[:]), r=[b_t0], w=[b_t0])
            P.op("dve", lambda eng: eng.tensor_scalar(out=mv[:, 0:1], in0=mv[:, 0:1], scalar1=rstd[:, 0:1], scalar2=-1.0,
                                                    op0=ALU.mult, op1=ALU.mult), r=[b_t0], w=[b_t0])
            P.op("act", lambda eng: eng.activation(out=t1[:], in_=t1[:], func=AF.Identity, bias=mv[:, 0:1], scale=rstd[:, 0:1]),
                 r=[b_t0, b_t1], w=[b_t1])
            P.op("dve", lambda eng: eng.tensor_tensor(out=t1[:], in0=t1[:], in1=lngbc[:], op=ALU.mult), r=[b_t1, b_bc], w=[b_t1])
            P.op("dve", lambda eng: eng.tensor_tensor(out=outt, in0=t1[:], in1=lnbbc[:], op=ALU.add), r=[b_t1, b_bc], w=[b_out])

        epsc = sb("epsc", [128, 1])
        P.op("dve", lambda eng: eng.memset(epsc[:], LN_EPS), w=[cst])

        def xtile_dram(kind, t):
            if kind == "x":
                return x_in.ap()[t * 128:(t + 1) * 128, :]
            if kind == "XA":
                return XA.ap()[t * 128:(t + 1) * 128, :]
            if kind == "XB":
                return XB.ap()[t * 128:(t + 1) * 128, :]
            if kind == "out":
                return out_t.ap()[t * 128:(t + 1) * 128, :]
            if kind == "X1B":
                B, j = t // NT2, t % NT2
                return X1B[j // 4].ap()[B * 128:(B + 1) * 128, (j % 4) * D:(j % 4 + 1) * D]
            raise ValueError(kind)

        def load_x(kind, t, tile_ap, b, eng="sp"):
            if kind == "X1Bown":
                g, jj = t // 4, t % 4
                src = X1B[g].ap()[:, jj * D:(jj + 1) * D]
                raise ValueError("use load_x_group")
            P.dma(eng, lambda e_: e_.dma_start(out=tile_ap, in_=xtile_dram(kind, t)), b, w=[b])

        def moe(l, T, C, ci, src_kind, dst_kind):
            NT = T // 128
            lj = 2 * l + 1
            NS = C // 128
            XSall, YSall = Buf("XS"), Buf("YS")
            with contextlib.ExitStack() as st:
                rw = sb("rw", [128, NCH, E], st=st)
                rbb = sb("rbb", [128, E], st=st)
                b_rw = Buf()
                P.dma("sp", lambda eng: eng.dma_start(out=rw[:], in_=router_w.ap()[l].rearrange("(k p) e -> p k e", p=128)),
                      b_rw, w=[b_rw])
                P.dma("sp", lambda eng: eng.dma_start(out=rbb[:], in_=router_b_bc.ap()[l]), b_rw, w=[b_rw])
                NB = 3
                xt = [sb("r_x%d" % i, [128, D], st=st) for i in range(NB)]
                b_xt = [Buf() for _ in range(NB)]
                hT = [sb("r_hT%d" % i, [128, NCH, 128], st=st) for i in range(2)]
                b_hT = [Buf() for _ in range(2)]
                row = [sb("r_row%d" % i, [128, ROWW], BF16, st=st) for i in range(NB)]
                b_row = [Buf() for _ in range(NB)]
                ptr = [ps("r_ptr%d" % i, [128, 512], st=st) for i in range(2)]
                b_ptr = [Buf() for _ in range(2)]
                plg = ps("r_plg", [128, 512], st=st)
                b_plg = Buf()
                prk = ps("r_prk", [128, 512], st=st)
                b_prk = Buf()
                lg = sb("r_lg", [128, E], st=st)
                v8 = sb("r_v8", [128, 8], st=st)
                msk = sb("r_msk", [128, E], st=st)
                msum = sb("r_msum", [128, E], st=st)
                ex = sb("r_ex", [128, E], st=st)
                ssum = sb("r_ssum", [128, 1], st=st)
                nmx = sb("r_nmx", [128, 1], st=st)
                sv = sb("r_sv", [128, E], st=st)
                sv8 = sb("r_sv8", [128, 8], st=st)
                b_s = Buf()
                b_msum = Buf()
                b_sidx = Buf()
                P.op("dve", lambda eng: eng.memset(msum[:], 0.0), w=[b_msum])
                for t in range(NT):
                    i3, i2 = t % NB, t % 2
                    load_x(src_kind, t, xt[i3][:], b_xt[i3])
                    for half in range(2):
                        pb = ptr[half]
                        for kk in range(4):
                            k = half * 4 + kk
                            P.op("pe", lambda eng, pb=pb, kk=kk, k=k, i3=i3: eng.transpose(
                                pb[:, kk * 128:(kk + 1) * 128], xt[i3][:, k * 128:(k + 1) * 128], ident_f[:]),
                                r=[b_xt[i3], cst], w=[b_ptr[half]])
                        for kk in range(4):
                            k = half * 4 + kk
                            P.op("act", lambda eng, pb=pb, kk=kk, k=k, i2=i2: eng.activation(
                                out=hT[i2][:, k, :], in_=pb[:, kk * 128:(kk + 1) * 128], func=AF.Identity,
                                bias=ada[:, lj, k:k + 1], scale=ada[:, lj, 8 + k:9 + k]), r=[b_ptr[half]], w=[b_hT[i2]])
                    for k in range(NCH):
                        P.op("pe", lambda eng, k=k, i2=i2: eng.matmul(plg[:, 0:E], lhsT=hT[i2][:, k, :], rhs=rw[:, k, :],
                                                                    start=(k == 0), stop=(k == NCH - 1)),
                             r=[b_hT[i2], b_rw], w=[b_plg])
                    P.op("dve", lambda eng: eng.tensor_tensor(out=lg[:], in0=plg[:, 0:E], in1=rbb[:], op=ALU.add),
                         r=[b_plg, b_rw], w=[b_s])
                    P.op("dve", lambda eng: eng.max(out=v8[:], in_=lg[:]), r=[b_s], w=[b_s])
                    P.op("dve", lambda eng: eng.tensor_scalar(out=msk[:], in0=lg[:], scalar1=v8[:, 3:4], scalar2=None,
                                                            op0=ALU.is_ge), r=[b_s], w=[b_s])
                    P.op("dve", lambda eng: eng.tensor_scalar(out=nmx[:], in0=v8[:, 0:1], scalar1=-1.0, scalar2=None,
                                                            op0=ALU.mult), r=[b_s], w=[b_s])
                    P.op("act", lambda eng: eng.activation(out=ex[:], in_=lg[:], func=AF.Exp, bias=nmx[:, 0:1], scale=1.0),
                         r=[b_s], w=[b_s])
                    P.op("dve", lambda eng: eng.tensor_tensor(out=ex[:], in0=ex[:], in1=msk[:], op=ALU.mult), r=[b_s], w=[b_s])
                    P.op("dve", lambda eng: eng.tensor_reduce(out=ssum[:], in_=ex[:], axis=AX.X, op=ALU.add), r=[b_s], w=[b_s])
                    P.op("dve", lambda eng: eng.reciprocal(out=ssum[:], in_=ssum[:]), r=[b_s], w=[b_s])
                    P.op("dve", lambda eng, i3=i3: eng.tensor_scalar(
                        out=row[i3][:, 1024:ROWW].bitcast(F32)[:, 0:E], in0=ex[:], scalar1=ssum[:, 0:1], scalar2=None, op0=ALU.mult),
                        r=[b_s], w=[b_row[i3]])
                    P.op("pool", lambda eng, i3=i3: eng.tensor_copy(out=row[i3][:, 0:1024], in_=xt[i3][:]),
                         r=[b_xt[i3]], w=[b_row[i3]])
                    P.op("pe", lambda eng: eng.matmul(prk[:, 0:E], lhsT=ustrict[:], rhs=msk[:], start=True, stop=False),
                         r=[b_s, cst], w=[b_prk])
                    P.op("pe", lambda eng: eng.matmul(prk[:, 0:E], lhsT=ones_f[:], rhs=msum[:], start=False, stop=True),
                         r=[b_msum, cst], w=[b_prk])
                    P.op("dve", lambda eng: eng.tensor_tensor(out=sv[:], in0=prk[:, 0:E], in1=ebase[:, ci, :], op=ALU.add),
                         r=[b_prk, cst], w=[b_s])
                    P.op("dve", lambda eng: eng.tensor_tensor(out=sv[:], in0=sv[:], in1=ebase[:, 2 + ci, :], op=ALU.min),
                         r=[b_s, cst], w=[b_s])
                    P.op("dve", lambda eng: eng.tensor_tensor(out=sv[:], in0=sv[:], in1=msk[:], op=ALU.mult), r=[b_s], w=[b_s])
                    P.op("dve", lambda eng: eng.tensor_tensor(out=msum[:], in0=msum[:], in1=msk[:], op=ALU.add),
                         r=[b_s, b_msum], w=[b_msum])
                    P.op("dve", lambda eng: eng.max(out=sv8[:], in_=sv[:]), r=[b_s], w=[b_s])
                    P.op("dve", lambda eng: eng.tensor_scalar(out=sv8[:, 0:4], in0=sv8[:, 0:4], scalar1=-1.0, scalar2=None,
                                                            op0=ALU.add), r=[b_s], w=[b_s])
                    P.op("dve", lambda eng, t=t: eng.tensor_copy(out=sidx[:, t, :], in_=sv8[:, 0:4]), r=[b_s], w=[b_sidx])
                    for kk in range(TOPK):
                        P.dma("pool", lambda eng, t=t, kk=kk, i3=i3: eng.indirect_dma_start(
                            out=XS.ap(), out_offset=bass.IndirectOffsetOnAxis(ap=sidx[:, t, kk:kk + 1], axis=0),
                            in_=row[i3][:], in_offset=None),
                            b_row[i3], r=[b_row[i3], b_sidx], acc=[XSall])
                if debug and "SIDX" in debug and l == 0:
                    P.dma("sp", lambda eng: eng.dma_start(out=SIDX.ap(), in_=sidx[:].rearrange("p a b -> p (a b)")), b_sidx, r=[b_sidx])
                P.flush()

            with contextlib.ExitStack() as st:
                NSB = (C + 511) // 512
                wgu = [sb("b_wgu%d" % i, [128, NCH, 2 * D], BF16, st=st) for i in range(2)]
                wdn = [sb("b_wdn%d" % i, [128, NCH, D], BF16, st=st) for i in range(2)]
                bdn = [sb("b_bdn%d" % i, [1, D], BF16, st=st) for i in range(2)]
                b_w = [Buf() for _ in range(2)]
                bgu = sb("b_bgu", [128, E, 16], st=st)
                b_bgu = Buf()
                P.dma("sp", lambda eng: eng.dma_start(out=bgu[:], in_=exp_b_guT.ap()[l]), b_bgu, w=[b_bgu])
                xr = [sb("b_xr%d" % i, [128, 4, ROWW], BF16, st=st) for i in range(3)]
                b_xr = [Buf() for _ in range(3)]
                xT = [sb("b_xT%d" % i, [128, NCH, 512], BF16, st=st) for i in range(2)]
                b_xT = [Buf() for _ in range(2)]
                aT = [sb("b_aT%d" % i, [128, NCH, 512], BF16, st=st) for i in range(2)]
                b_aT = [Buf() for _ in range(2)]
                gs = [sb("b_g%d" % i, [128, 512], st=st) for i in range(2)]
                sg = [sb("b_sg%d" % i, [128, 512], st=st) for i in range(2)]
                us = [sb("b_u%d" % i, [128, 512], st=st) for i in range(2)]
                b_g = [Buf() for _ in range(2)]
                b_sg = [Buf() for _ in range(2)]
                b_u = [Buf() for _ in range(2)]
                yo = [sb("b_y%d" % i, [128, D], st=st) for i in range(2)]
                b_yo = [Buf() for _ in range(2)]
                onesb = sb("b_ones", [1, 128], BF16, st=st)
                P.op("dve", lambda eng: eng.memset(onesb[:], 1.0), w=[cst])
                ptr = [ps("b_ptr%d" % i, [128, 1024], BF16, st=st) for i in range(1)]
                b_ptr = [Buf() for _ in range(1)]
                pg = [ps("b_pg%d" % i, [128, 512], st=st) for i in range(2)]
                pu = [ps("b_pu%d" % i, [128, 512], st=st) for i in range(2)]
                b_pg = [Buf() for _ in range(2)]
                b_pu = [Buf() for _ in range(2)]
                py = [ps("b_py%d" % i, [128, 512], st=st) for i in range(3)]
                b_py = [Buf() for _ in range(3)]

                def load_expert(e):
                    i = e % 2
                    load_w_bf16(wgu[i][:], exp_w_gu.ap()[l, e], b_w[i])
                    load_w_bf16(wdn[i][:], exp_w_down.ap()[l, e], b_w[i])
                    P.dma("pool", lambda eng: eng.dma_start(out=bdn[i][:], in_=exp_b_down.ap()[l, e:e + 1, :]), b_w[i], acc=[b_w[i]])

                blocks = []
                for e in range(E):
                    for sbk in range(NSB):
                        blocks.append((e, sbk, len(blocks) % 2, len(blocks) % 3))
                state = {"y": 0, "py": 0}

                def emit_T(blk):
                    e, sbk, j, j3 = blk
                    n = min(512, C - sbk * 512)
                    nt = n // 128
                    s0 = e * C + sbk * 512
                    P.dma("sp", lambda eng: eng.dma_start(
                        out=xr[j3][:, 0:nt, :], in_=XS.ap()[s0:s0 + n, :].rearrange("(t p) c -> p t c", p=128)),
                        b_xr[j3], r=[XSall], w=[b_xr[j3]])
                    for k in range(NCH):
                        for tt in range(nt):
                            P.op("pe", lambda eng, k=k, tt=tt: eng.transpose(
                                ptr[0][:, tt * 128:(tt + 1) * 128], xr[j3][:, tt, k * 128:(k + 1) * 128], ident_b[:]),
                                r=[b_xr[j3], cst], w=[b_ptr[0]])
                        P.op("act", lambda eng, k=k: eng.activation(
                            out=xT[j][:, k, 0:n], in_=ptr[0][:, 0:n], func=AF.Identity, bias=ada[:, lj, k:k + 1],
                            scale=ada[:, lj, 8 + k:9 + k]), r=[b_ptr[0]], w=[b_xT[j]])

                def emit_GU(blk):
                    e, sbk, j, j3 = blk
                    i = e % 2
                    n = min(512, C - sbk * 512)
                    for f in range(NCH):
                        q2 = f % 2
                        for k in range(NCH):
                            P.op("pe", lambda eng, f=f, k=k, q2=q2: eng.matmul(
                                pg[q2][:, 0:n], lhsT=wgu[i][:, k, f * 128:(f + 1) * 128], rhs=xT[j][:, k, 0:n],
                                start=(k == 0), stop=(k == NCH - 1)), r=[b_w[i], b_xT[j]], w=[b_pg[q2]])
                        for k in range(NCH):
                            P.op("pe", lambda eng, f=f, k=k, q2=q2: eng.matmul(
                                pu[q2][:, 0:n], lhsT=wgu[i][:, k, D + f * 128:D + (f + 1) * 128], rhs=xT[j][:, k, 0:n],
                                start=(k == 0), stop=(k == NCH - 1)), r=[b_w[i], b_xT[j]], w=[b_pu[q2]])
                        P.op("dve", lambda eng, f=f, q2=q2: eng.tensor_scalar(
                            out=gs[q2][:, 0:n], in0=pg[q2][:, 0:n], scalar1=bgu[:, e, f:f + 1], scalar2=LIMIT,
                            op0=ALU.add, op1=ALU.min), r=[b_pg[q2], b_bgu], w=[b_g[q2]])
                        P.op("act", lambda eng, q2=q2: eng.activation(out=sg[q2][:, 0:n], in_=gs[q2][:, 0:n],
                                                                     func=AF.Sigmoid, scale=SW_ALPHA),
                             r=[b_g[q2]], w=[b_sg[q2]])
                        P.op("dve", lambda eng, f=f, q2=q2: eng.tensor_scalar(
                            out=us[q2][:, 0:n], in0=pu[q2][:, 0:n], scalar1=bgu[:, e, 8 + f:9 + f], scalar2=LIMIT,
                            op0=ALU.add, op1=ALU.min), r=[b_pu[q2], b_bgu], w=[b_u[q2]])
                        P.op("pool", lambda eng, q2=q2: eng.tensor_scalar(
                            out=us[q2][:, 0:n], in0=us[q2][:, 0:n], scalar1=-LIMIT, scalar2=1.0, op0=ALU.max, op1=ALU.add),
                            r=[b_u[q2]], w=[b_u[q2]])
                        P.op("pool", lambda eng, q2=q2: eng.tensor_tensor(out=gs[q2][:, 0:n], in0=gs[q2][:, 0:n],
                                                                         in1=sg[q2][:, 0:n], op=ALU.mult),
                             r=[b_g[q2], b_sg[q2]], w=[b_g[q2]])
                        P.op("dve", lambda eng, f=f, q2=q2: eng.tensor_tensor(
                            out=aT[j][:, f, 0:n], in0=gs[q2][:, 0:n], in1=us[q2][:, 0:n], op=ALU.mult),
                            r=[b_g[q2], b_u[q2]], w=[b_aT[j]])

                def emit_D(blk):
                    e, sbk, j, j3 = blk
                    i = e % 2
                    n = min(512, C - sbk * 512)
                    nt = n // 128
                    s0 = e * C + sbk * 512
                    for tt in range(nt):
                        y2 = state["y"] % 2
                        state["y"] += 1
                        for hh in range(2):
                            pi = state["py"] % 3
                            state["py"] += 1
                            for f in range(NCH):
                                P.op("pe", lambda eng, tt=tt, hh=hh, f=f, pi=pi: eng.matmul(
                                    py[pi][:, :], lhsT=aT[j][:, f, tt * 128:(tt + 1) * 128],
                                    rhs=wdn[i][:, f, hh * 512:(hh + 1) * 512], start=(f == 0), stop=False),
                                    r=[b_aT[j], b_w[i]], w=[b_py[pi]])
                            P.op("pe", lambda eng, hh=hh, pi=pi: eng.matmul(
                                py[pi][:, :], lhsT=onesb[:], rhs=bdn[i][:, hh * 512:(hh + 1) * 512],
                                start=False, stop=True), r=[b_w[i], cst], w=[b_py[pi]])
                            P.op("act", lambda eng, tt=tt, hh=hh, pi=pi, y2=y2: eng.activation(
                                out=yo[y2][:, hh * 512:(hh + 1) * 512], in_=py[pi][:, :], func=AF.Copy,
                                scale=xr[j3][:, tt, 1024:ROWW].bitcast(F32)[:, e:e + 1]), r=[b_py[pi], b_xr[j3]], w=[b_yo[y2]])
                        r0 = s0 + tt * 128
                        P.dma("sp", lambda eng, y2=y2, r0=r0: eng.dma_start(out=YS.ap()[r0:r0 + 128, :], in_=yo[y2][:]),
                              b_yo[y2], r=[b_yo[y2]], acc=[YSall])

                load_expert(0)
                if E > 1:
                    load_expert(1)
                nb = len(blocks)
                emit_T(blocks[0])
                emit_GU(blocks[0])
                if nb > 1:
                    emit_T(blocks[1])
                for bi, blk in enumerate(blocks):
                    e, sbk, j, j3 = blk
                    if bi + 1 < nb:
                        emit_GU(blocks[bi + 1])
                    if bi + 2 < nb:
                        emit_T(blocks[bi + 2])
                    emit_D(blk)
                    if sbk == NSB - 1 and e + 2 < E:
                        load_expert(e + 2)
                P.flush()

            with contextlib.ExitStack() as st:
                g1bc = sb("c_g1", [128, D], st=st)
                lng = sb("c_lng", [128, D], st=st)
                lnb = sb("c_lnb", [128, D], st=st)
                b_bc = Buf()
                pbc = ps("c_pbc", [128, 512], st=st)
                bc_from_cols(st, "c_g1", lambda k: ada[:, lj, 16 + k:17 + k], pbc, g1bc, b_bc)
                P.dma("sp", lambda eng: eng.dma_start(out=lng[:], in_=ln_g_bc.ap()[lj]), b_bc, acc=[b_bc])
                P.dma("sp", lambda eng: eng.dma_start(out=lnb[:], in_=ln_b_bc.ap()[lj]), b_bc, acc=[b_bc])
                NB = 2
                xt = [sb("c_x%d" % i, [128, D], st=st) for i in range(NB)]
                yg = [sb("c_yg%d" % i, [128, 4, D], st=st) for i in range(NB)]
                t0 = [sb("c_t0%d" % i, [128, D], st=st) for i in range(NB)]
                t1 = [sb("c_t1%d" % i, [128, D], st=st) for i in range(NB)]
                xo = [sb("c_xo%d" % i, [128, D], st=st) for i in range(NB)]
                small = [(sb("c_st%d" % i, [128, 2, 6], st=st), sb("c_mv%d" % i, [128, 2], st=st),
                          sb("c_rs%d" % i, [128, 1], st=st)) for i in range(NB)]
                b_xt = [Buf() for _ in range(NB)]
                b_yg = [Buf() for _ in range(NB)]
                b_t0 = [Buf() for _ in range(NB)]
                b_t1 = [Buf() for _ in range(NB)]
                b_xo = [Buf() for _ in range(NB)]
                for t in range(NT):
                    i = t % NB
                    load_x(src_kind, t, xt[i][:], b_xt[i])
                    for kk in range(TOPK):
                        P.dma("pool", lambda eng, t=t, kk=kk, i=i: eng.indirect_dma_start(
                            out=yg[i][:, kk, :], out_offset=None, in_=YS.ap(),
                            in_offset=bass.IndirectOffsetOnAxis(ap=sidx[:, t, kk:kk + 1], axis=0)),
                            b_yg[i], r=[YSall], acc=[b_yg[i]])
                    P.op("dve", lambda eng, i=i: eng.tensor_tensor(out=yg[i][:, 0, :], in0=yg[i][:, 0, :], in1=yg[i][:, 1, :],
                                                                 op=ALU.add), r=[b_yg[i]], w=[b_yg[i]])
                    P.op("pool", lambda eng, i=i: eng.tensor_tensor(out=yg[i][:, 2, :], in0=yg[i][:, 2, :], in1=yg[i][:, 3, :],
                                                                  op=ALU.add), r=[b_yg[i]], w=[b_yg[i]])
                    P.op("dve", lambda eng, i=i: eng.tensor_tensor(out=yg[i][:, 0, :], in0=yg[i][:, 0, :], in1=yg[i][:, 2, :],
                                                                 op=ALU.add), r=[b_yg[i]], w=[b_yg[i]])
                    residual_ln(xt[i][:], [(yg[i][:, 0, :], 0, D)], g1bc, lng, lnb, t0[i], t1[i], xo[i][:],
                                b_xt[i], b_yg[i], b_bc, b_t0[i], b_t1[i], b_xo[i], small[i])
                    P.dma("sp", lambda eng, t=t, i=i: eng.dma_start(out=xtile_dram(dst_kind, t), in_=xo[i][:]),
                          b_xo[i], r=[b_xo[i]])
                P.flush()

        def conv_layer(l, src_kind):
            lj = 2 * l
            with contextlib.ExitStack() as st:
                win = sb("a_win", [128, NCH, 3 * D], BF16, st=st)
                wout = sb("a_wout", [128, NCH, D], BF16, st=st)
                b_w = Buf()
                load_w_bf16(win[:], conv_w_in.ap()[l], b_w)
                P.dma("pool", lambda eng: eng.dma_start(out=wout[:], in_=conv_w_out.ap()[l].rearrange("(k p) n -> p k n", p=128)),
                      b_w, acc=[b_w])
                g1bc = sb("a_g1", [128, D], st=st)
                lng = sb("a_lng", [128, D], st=st)
                lnb = sb("a_lnb", [128, D], st=st)
                b_bc = Buf()
                pbc = ps("a_pbc", [128, 512], st=st)
                bc_from_cols(st, "a_g1", lambda k: ada[:, lj, 16 + k:17 + k], pbc, g1bc, b_bc)
                P.dma("sp", lambda eng: eng.dma_start(out=lng[:], in_=ln_g_bc.ap()[lj]), b_bc, acc=[b_bc])
                P.dma("sp", lambda eng: eng.dma_start(out=lnb[:], in_=ln_b_bc.ap()[lj]), b_bc, acc=[b_bc])
                xt = [sb("a_x%d" % i, [128, 4, D], st=st) for i in range(2)]
                b_xt = [Buf() for _ in range(2)]
                hT = [sb("a_hT%d" % i, [128, NCH, 512], BF16, st=st) for i in range(2)]
                b_hT = [Buf() for _ in range(2)]
                z = sb("a_z", [128, NCH, 514], st=st)
                b_z = [Buf() for _ in range(NCH)]
                gcs = [sb("a_gc%d" % i, [128, 512], st=st) for i in range(2)]
                b_gcs = [Buf() for _ in range(2)]
                zc = [sb("a_zc%d" % i, [128, 512], st=st) for i in range(2)]
                b_zc = [Buf() for _ in range(2)]
                vT = [sb("a_vT%d" % i, [128, NCH, 512], BF16, st=st) for i in range(2)]
                b_vT = [Buf() for _ in range(2)]
                t0 = [sb("a_t0%d" % i, [128, D], st=st) for i in range(2)]
                t1 = [sb("a_t1%d" % i, [128, D], st=st) for i in range(2)]
                xo = [sb("a_xo%d" % i, [128, D], st=st) for i in range(2)]
                small = [(sb("a_st%d" % i, [128, 2, 6], st=st), sb("a_mv%d" % i, [128, 2], st=st),
                          sb("a_rs%d" % i, [128, 1], st=st)) for i in range(2)]
                b_t0 = [Buf() for _ in range(2)]
                b_t1 = [Buf() for _ in range(2)]
                b_xo = [Buf() for _ in range(2)]
                ptr = [ps("a_ptr%d" % i, [128, 512], st=st) for i in range(2)]
                b_ptr = [Buf() for _ in range(2)]
                pc = ps("a_pc", [128, 512], st=st)
                pb_ = ps("a_pb", [128, 512], st=st)
                pu = ps("a_pu", [128, 512], st=st)
                b_pc, b_pb, b_pu = Buf(), Buf(), Buf()
                po = ps("a_po", [128, 1024], st=st)
                b_po = Buf()
                P.op("dve", lambda eng: eng.memset(z[:], 0.0), w=b_z)
                NG = T1 // 512
                tcnt = 0
                for g in range(NG):
                    gi = g % 2
                    for tt in range(4):
                        t = g * 4 + tt
                        P.dma("sp", lambda eng, gi=gi, tt=tt, t=t: eng.dma_start(out=xt[gi][:, tt, :], in_=xtile_dram(src_kind, t)),
                              b_xt[gi], acc=[b_xt[gi]], w=[])
                    for k in range(NCH):
                        pi = k % 2
                        for tt in range(4):
                            P.op("pe", lambda eng, k=k, tt=tt, gi=gi, pi=pi: eng.transpose(
                                ptr[pi][:, tt * 128:(tt + 1) * 128], xt[gi][:, tt, k * 128:(k + 1) * 128], ident_f[:]),
                                r=[b_xt[gi], cst], w=[b_ptr[pi]])
                        P.op("act", lambda eng, k=k, gi=gi, pi=pi: eng.activation(
                            out=hT[gi][:, k, :], in_=ptr[pi][:], func=AF.Identity, bias=ada[:, lj, k:k + 1],
                            scale=ada[:, lj, 8 + k:9 + k]), r=[b_ptr[pi]], w=[b_hT[gi]])
                    for f in range(NCH):
                        q2 = f % 2
                        for (pp, bb, off) in [(pc, b_pc, 0), (pu, b_pu, 2 * D), (pb_, b_pb, D)]:
                            for k in range(NCH):
                                P.op("pe", lambda eng, pp=pp, off=off, f=f, k=k, gi=gi: eng.matmul(
                                    pp[:], lhsT=win[:, k, off + f * 128:off + (f + 1) * 128], rhs=hT[gi][:, k, :],
                                    start=(k == 0), stop=(k == NCH - 1)), r=[b_w, b_hT[gi]], w=[bb])
                        P.op("act", lambda eng, q2=q2: eng.activation(out=gcs[q2][:], in_=pc[:], func=AF.Copy),
                             r=[b_pc], w=[b_gcs[q2]])
                        P.op("dve", lambda eng, f=f, q2=q2: eng.tensor_tensor(out=z[:, f, 2:514], in0=pu[:], in1=gcs[q2][:],
                                                                            op=ALU.mult), r=[b_pu, b_gcs[q2]], w=[b_z[f]])
                        P.op("pool", lambda eng, f=f, q2=q2: eng.tensor_scalar(
                            out=zc[q2][:], in0=z[:, f, 2:514], scalar1=convw[:, l, f, 2:3], scalar2=None, op0=ALU.mult),
                            r=[b_z[f], cst], w=[b_zc[q2]])
                        P.op("dve", lambda eng, f=f, q2=q2: eng.scalar_tensor_tensor(
                            out=zc[q2][:], in0=z[:, f, 1:513], scalar=convw[:, l, f, 1:2], in1=zc[q2][:], op0=ALU.mult,
                            op1=ALU.add), r=[b_z[f], b_zc[q2], cst], w=[b_zc[q2]])
                        P.op("dve", lambda eng, f=f, q2=q2: eng.scalar_tensor_tensor(
                            out=zc[q2][:], in0=z[:, f, 0:512], scalar=convw[:, l, f, 0:1], in1=zc[q2][:], op0=ALU.mult,
                            op1=ALU.add), r=[b_z[f], b_zc[q2], cst], w=[b_zc[q2]])
                        P.op("dve", lambda eng, f=f, q2=q2, gi=gi: eng.tensor_tensor(
                            out=vT[gi][:, f, :], in0=pb_[:], in1=zc[q2][:], op=ALU.mult), r=[b_pb, b_zc[q2]], w=[b_vT[gi]])
                        P.op("pool", lambda eng, f=f: eng.tensor_copy(out=z[:, f, 0:2], in_=z[:, f, 512:514]),
                             r=[b_z[f]], w=[b_z[f]])
                    for tt in range(4):
                        t = g * 4 + tt
                        i = tcnt % 2
                        tcnt += 1
                        for hh in range(2):
                            for f in range(NCH):
                                P.op("pe", lambda eng, tt=tt, hh=hh, f=f, gi=gi: eng.matmul(
                                    po[:, hh * 512:(hh + 1) * 512], lhsT=vT[gi][:, f, tt * 128:(tt + 1) * 128],
                                    rhs=wout[:, f, hh * 512:(hh + 1) * 512], start=(f == 0), stop=(f == NCH - 1)),
                                    r=[b_vT[gi], b_w], w=[b_po])
                        residual_ln(xt[gi][:, tt, :], [(po[:, 0:512], 0, 512), (po[:, 512:1024], 512, 512)], g1bc, lng, lnb,
                                    t0[i], t1[i], xo[i][:], b_xt[gi], b_po, b_bc, b_t0[i], b_t1[i], b_xo[i], small[i])
                        P.dma("sp", lambda eng, t=t, i=i: eng.dma_start(out=xtile_dram("XA", t), in_=xo[i][:]),
                              b_xo[i], r=[b_xo[i]])
                P.flush()

        def kv_stage():
            with contextlib.ExitStack() as st:
                wk = sb("k_w", [128, NCH, 2 * D + H], BF16, st=st)
                b_w = Buf()
                load_w_bf16(wk[:], w_kvf.ap(), b_w)
                zt = sb("k_zt", [128, T2], BF16, st=st)
                zf = sb("k_zf", [128, 512], st=st)
                b_z = Buf()
                P.op("dve", lambda eng: eng.memset(zt[:], 0.0), w=[b_z])
                P.op("pool", lambda eng: eng.memset(zf[:], 0.0), w=[b_z])
                for h in range(H):
                    P.dma("sp", lambda eng, h=h: eng.dma_start(out=KT[h].ap()[128:192, :], in_=zt[0:64, :]), b_z, r=[b_z])
                    P.dma("sp", lambda eng, h=h: eng.dma_start(out=VA[h].ap()[256:384, :], in_=zt[:, :]), b_z, r=[b_z])
                P.dma("sp", lambda eng: eng.dma_start(out=LF.ap()[256:384, :], in_=zf[:, 0:NT2 * H]), b_z, r=[b_z])
                for cc in range(T2 // 512):
                    P.dma("sp", lambda eng, cc=cc: eng.dma_start(out=LFT.ap()[32:48, cc * 512:(cc + 1) * 512], in_=zf[0:16, :]),
                          b_z, r=[b_z])
                xt = [sb("k_x%d" % i, [128, 4, D], st=st) for i in range(2)]
                b_xt = [Buf() for _ in range(2)]
                hT = [sb("k_hT%d" % i, [128, NCH, 512], BF16, st=st) for i in range(2)]
                b_hT = [Buf() for _ in range(2)]
                kst = [sb("k_kst%d" % i, [64, H, 512], BF16, st=st) for i in range(2)]
                b_kst = [Buf() for _ in range(2)]
                vst = [sb("k_vst%d" % i, [128, H, 128], BF16, st=st) for i in range(2)]
                b_vst = [Buf() for _ in range(2)]
                for i in range(2):
                    P.op("dve", lambda eng, i=i: eng.memset(vst[i][:], 1.0), w=[b_vst[i]])
                lfall = sb("k_lfall", [128, NT1, H], st=st)
                lftg = [sb("k_lftg%d" % i, [16, 512], st=st) for i in range(2)]
                b_lfall = Buf()
                b_lftg = [Buf() for _ in range(2)]
                zl = sb("k_zl", [128, H], st=st)
                l1 = sb("k_l1", [128, H], st=st)
                lsum = sb("k_lsum", [128, H], st=st)
                lfc = sb("k_lfc", [128, H], st=st)
                b_s, b_lsum = Buf(), Buf()
                P.op("dve", lambda eng: eng.memset(lsum[:], 0.0), w=[b_lsum])
                ptr = [ps("k_ptr%d" % i, [128, 512], st=st) for i in range(2)]
                b_ptr = [Buf() for _ in range(2)]
                pk = [ps("k_pk%d" % i, [128, 512], st=st) for i in range(2)]
                b_pk = [Buf() for _ in range(2)]
                pv = ps("k_pv", [128, 1024], st=st)
                b_pv = Buf()
                pf = ps("k_pf", [128, 512], st=st)
                b_pf = Buf()
                vcnt = 0
                for g in range(T1 // 512):
                    gi = g % 2
                    B, gl = g // NGB, g % NGB
                    for tt in range(4):
                        t = g * 4 + tt
                        P.dma("sp", lambda eng, gi=gi, tt=tt, t=t: eng.dma_start(out=xt[gi][:, tt, :], in_=xtile_dram("X1B", t)),
                              b_xt[gi], acc=[b_xt[gi]])
                    for k in range(NCH):
                        pi = k % 2
                        for tt in range(4):
                            P.op("pe", lambda eng, k=k, tt=tt, gi=gi, pi=pi: eng.transpose(
                                ptr[pi][:, tt * 128:(tt + 1) * 128], xt[gi][:, tt, k * 128:(k + 1) * 128], ident_f[:]),
                                r=[b_xt[gi], cst], w=[b_ptr[pi]])
                        P.op("act", lambda eng, k=k, gi=gi, pi=pi: eng.activation(
                            out=hT[gi][:, k, :], in_=ptr[pi][:], func=AF.Identity, bias=kvada[:, k:k + 1],
                            scale=kvada[:, 8 + k:9 + k]), r=[b_ptr[pi]], w=[b_hT[gi]])
                    for h in range(H):
                        q2 = h % 2
                        for k in range(NCH):
                            P.op("pe", lambda eng, h=h, k=k, q2=q2, gi=gi: eng.matmul(
                                pk[q2][0:64, :], lhsT=wk[:, k, h * 64:(h + 1) * 64], rhs=hT[gi][:, k, :],
                                start=(k == 0), stop=(k == NCH - 1)), r=[b_w, b_hT[gi]], w=[b_pk[q2]])
                        P.op("act", lambda eng, h=h, q2=q2, gi=gi: eng.activation(out=kst[gi][:, h, :], in_=pk[q2][0:64, :],
                                                                                func=AF.Copy), r=[b_pk[q2]], w=[b_kst[gi]])
                    for h in range(H):
                        P.dma("sp", lambda eng, h=h, gi=gi, B=B, gl=gl: eng.dma_start(
                            out=KT[h].ap()[B * 64:(B + 1) * 64, gl * 512:(gl + 1) * 512], in_=kst[gi][:, h, :]),
                            b_kst[gi], r=[b_kst[gi]])
                    for tt in range(4):
                        t = g * 4 + tt
                        j = gl * 4 + tt
                        vi = vcnt % 2
                        vcnt += 1
                        for hh in range(2):
                            for k in range(NCH):
                                P.op("pe", lambda eng, tt=tt, hh=hh, k=k, gi=gi: eng.matmul(
                                    pv[:, hh * 512:(hh + 1) * 512], lhsT=hT[gi][:, k, tt * 128:(tt + 1) * 128],
                                    rhs=wk[:, k, D + hh * 512:D + (hh + 1) * 512], start=(k == 0), stop=(k == NCH - 1)),
                                    r=[b_w, b_hT[gi]], w=[b_pv])
                        P.op("act", lambda eng, vi=vi: eng.activation(out=vst[vi][:, :, 0:64],
                                                                     in_=pv[:].rearrange("p (h d) -> p h d", d=64),
                                                                     func=AF.Copy), r=[b_pv], w=[b_vst[vi]])
                        for h in range(H):
                            P.dma("sp", lambda eng, h=h, vi=vi, B=B, j=j: eng.dma_start(
                                out=VA[h].ap()[B * 128:(B + 1) * 128, j * 128:(j + 1) * 128], in_=vst[vi][:, h, :]),
                                b_vst[vi], r=[b_vst[vi]])
                        for k in range(NCH):
                            P.op("pe", lambda eng, tt=tt, k=k, gi=gi: eng.matmul(
                                pf[:, 0:H], lhsT=hT[gi][:, k, tt * 128:(tt + 1) * 128], rhs=wk[:, k, 2 * D:2 * D + H],
                                start=(k == 0), stop=(k == NCH - 1)), r=[b_w, b_hT[gi]], w=[b_pf])
                        P.op("dve", lambda eng: eng.tensor_tensor(out=zl[:], in0=pf[:, 0:H], in1=bfb[:], op=ALU.add),
                             r=[b_pf, cst], w=[b_s])
                        P.op("act", lambda eng: eng.activation(out=zl[:], in_=zl[:], func=AF.Exp, scale=-1.0), r=[b_s], w=[b_s])
                        P.op("act", lambda eng: eng.activation(out=l1[:], in_=zl[:], func=AF.Ln, bias=1.0, scale=1.0),
                             r=[b_s], w=[b_s])
                        P.op("pe", lambda eng: eng.matmul(pf[:, 64:64 + H], lhsT=uincl[:], rhs=l1[:], start=True, stop=False),
                             r=[b_s, cst], w=[b_pf])
                        P.op("pe", lambda eng: eng.matmul(pf[:, 64:64 + H], lhsT=ones_f[:], rhs=lsum[:], start=False, stop=True),
                             r=[b_lsum, cst], w=[b_pf])
                        P.op("dve", lambda eng: eng.tensor_tensor(out=lsum[:], in0=lsum[:], in1=l1[:], op=ALU.add),
                             r=[b_s, b_lsum], w=[b_lsum])
                        P.op("dve", lambda eng, t=t: eng.tensor_scalar(out=lfall[:, t, :], in0=pf[:, 64:64 + H], scalar1=-1.0,
                                                                     scalar2=None, op0=ALU.mult), r=[b_pf], acc=[b_lfall])
                        P.op("pe", lambda eng, t=t: eng.transpose(pf[0:16, 128:256], lfall[:, t, :], ident_f[:]),
                             r=[b_lfall, cst], w=[b_pf])
                        P.op("act", lambda eng, tt=tt, gi=gi: eng.activation(out=lftg[gi][:, tt * 128:(tt + 1) * 128],
                                                                            in_=pf[0:16, 128:256], func=AF.Copy),
                             r=[b_pf], w=[b_lftg[gi]])
                    P.dma("sp", lambda eng, gi=gi, B=B, gl=gl: eng.dma_start(
                        out=LFT.ap()[B * 16:(B + 1) * 16, gl * 512:(gl + 1) * 512], in_=lftg[gi][:]), b_lftg[gi], r=[b_lftg[gi]])
                for B in range(2):
                    P.dma("sp", lambda eng, B=B: eng.dma_start(
                        out=LF.ap()[B * 128:(B + 1) * 128, :], in_=lfall[:, B * NT2:(B + 1) * NT2, :].rearrange("p a b -> p (a b)")),
                        b_lfall, r=[b_lfall])
                P.flush()

        def load_own_group(l, g, tile4, b, bufacc):
            if l == 2:
                P.dma("pool", lambda eng: eng.indirect_dma_start(
                    out=tile4[:].rearrange("p a b -> p (a b)"), out_offset=None, in_=X1B[g].ap(),
                    in_offset=bass.IndirectOffsetOnAxis(ap=idxs[:, 0:1], axis=0)), b, r=[cst], w=[b])
            else:
                for tt in range(4):
                    P.dma("sp", lambda eng, tt=tt: eng.dma_start(out=tile4[:, tt, :], in_=xtile_dram("XB", g * 4 + tt)),
                          b, acc=[b])

        def attn_layer(l):
            lj = 2 * l
            ja = l - 2
            with contextlib.ExitStack() as st:
                wq = sb("q_w", [128, NCH, D], BF16, st=st)
                b_w = Buf()
                load_w_bf16(wq[:], attn_w_q.ap()[ja], b_w)
                lft = sb("q_lft", [16, T2], st=st)
                b_lft = Buf()
                P.dma("pool", lambda eng: eng.indirect_dma_start(
                    out=lft[:], out_offset=None, in_=LFT.ap(), in_offset=bass.IndirectOffsetOnAxis(ap=idx16[0:16, 0:1], axis=0)),
                    b_lft, r=[cst], w=[b_lft])
                xt = [sb("q_x%d" % i, [128, 4, D], st=st) for i in range(2)]
                b_xt = [Buf() for _ in range(2)]
                hT = [sb("q_hT%d" % i, [128, NCH, 512], BF16, st=st) for i in range(2)]
                b_hT = [Buf() for _ in range(2)]
                qa = [sb("q_qa%d" % i, [128, 512], BF16, st=st) for i in range(3)]
                b_qa = [Buf() for _ in range(3)]
                t96 = sb("q_t96", [128, 512], BF16, st=st)
                b_t96 = Buf()
                for i in range(3):
                    P.op("dve", lambda eng, i=i: eng.memset(qa[i][:], 0.0), w=[b_qa[i]])
                ptr = [ps("q_ptr%d" % i, [128, 512], st=st) for i in range(2)]
                b_ptr = [Buf() for _ in range(2)]
                pq = [ps("q_pq%d" % i, [128, 512], st=st) for i in range(2)]
                b_pq = [Buf() for _ in range(2)]
                pa = [ps("q_pa%d" % i, [128, 512], st=st) for i in range(2)]
                b_pa = [Buf() for _ in range(2)]
                qc = 0
                for g in range(NGB):
                    gi = g % 2
                    load_own_group(l, g, xt[gi], b_xt[gi], None)
                    for k in range(NCH):
                        pi = k % 2
                        for tt in range(4):
                            P.op("pe", lambda eng, k=k, tt=tt, gi=gi, pi=pi: eng.transpose(
                                ptr[pi][:, tt * 128:(tt + 1) * 128], xt[gi][:, tt, k * 128:(k + 1) * 128], ident_f[:]),
                                r=[b_xt[gi], cst], w=[b_ptr[pi]])
                        P.op("act", lambda eng, k=k, gi=gi, pi=pi: eng.activation(
                            out=hT[gi][:, k, :], in_=ptr[pi][:], func=AF.Identity, bias=ada[:, lj, k:k + 1],
                            scale=ada[:, lj, 8 + k:9 + k]), r=[b_ptr[pi]], w=[b_hT[gi]])
                    for h in range(H):
                        q2 = h % 2
                        qi = qc % 3
                        qc += 1
                        for k in range(NCH):
                            P.op("pe", lambda eng, h=h, k=k, q2=q2, gi=gi: eng.matmul(
                                pq[q2][0:64, :], lhsT=wq[:, k, h * 64:(h + 1) * 64], rhs=hT[gi][:, k, :],
                                start=(k == 0), stop=(k == NCH - 1)), r=[b_w, b_hT[gi]], w=[b_pq[q2]])
                        P.op("pe", lambda eng, h=h, q2=q2, g=g: eng.matmul(
                            pa[q2][:, :], lhsT=selh[:, h, :], rhs=lft[:, g * 512:(g + 1) * 512], start=True, stop=True),
                            r=[b_lft, cst], w=[b_pa[q2]])
                        P.op("act", lambda eng, q2=q2, qi=qi: eng.activation(out=qa[qi][0:64, :], in_=pq[q2][0:64, :], func=AF.Copy),
                             r=[b_pq[q2]], w=[b_qa[qi]])
                        P.op("act", lambda eng, q2=q2, qi=qi: eng.activation(out=qa[qi][64:65, :], in_=pa[q2][64:65, :],
                                                                            func=AF.Copy, scale=1.0 / SCALE),
                             r=[b_pa[q2]], w=[b_qa[qi]])
                        P.op("act", lambda eng, q2=q2: eng.activation(out=t96[96:97, :], in_=pa[q2][96:97, :], func=AF.Copy,
                                                                     scale=1.0 / SCALE), r=[b_pa[q2]], w=[b_t96])
                        P.op("dve", lambda eng, q2=q2, qi=qi: eng.scalar_tensor_tensor(
                            out=qa[qi][96:97, :], in0=pa[q2][96:97, :], scalar=1.0 / SCALE, in1=t96[96:97, :], op0=ALU.mult,
                            op1=ALU.subtract), r=[b_pa[q2], b_t96], w=[b_qa[qi]])
                        P.dma("sp", lambda eng, h=h, g=g, qi=qi: eng.dma_start(out=QT.ap()[h, :, g * 512:(g + 1) * 512],
                                                                              in_=qa[qi][:, :]), b_qa[qi], r=[b_qa[qi]])
                P.flush()

            with contextlib.ExitStack() as st:
                kt = [sb("t_kt%d" % i, [128, 2 * T2], BF16, st=st) for i in range(2)]
                va = [sb("t_va%d" % i, [128, 2 * NT2, 128], BF16, st=st) for i in range(2)]
                qt = [sb("t_qt%d" % i, [128, T2], BF16, st=st) for i in range(2)]
                b_kt = [Buf() for _ in range(2)]
                b_va = [Buf() for _ in range(2)]
                b_qt = [Buf() for _ in range(2)]
                b_aug = [Buf() for _ in range(2)]
                for i in range(2):
                    P.op("pool", lambda eng, i=i: eng.memset(kt[i][64:128, :], 0.0), w=[b_aug[i]])
                    P.op("pool", lambda eng, i=i: eng.memset(kt[i][64:65, :], 1.0), w=[b_aug[i]])
                    P.op("pool", lambda eng, i=i: eng.memset(kt[i][96:97, :], 1.0), w=[b_aug[i]])
                nlf = sb("t_nlf", [128, 2, NT2, H], st=st)
                b_nlf = Buf()
                for blk, col in [(0, 1), (1, 0)]:
                    P.dma("pool", lambda eng, blk=blk, col=col: eng.indirect_dma_start(
                        out=nlf[:, blk, :, :].rearrange("p a b -> p (a b)"), out_offset=None, in_=LF.ap(),
                        in_offset=bass.IndirectOffsetOnAxis(ap=idxs[:, col:col + 1], axis=0)), b_nlf, r=[cst], acc=[b_nlf])
                P.op("dve", lambda eng: eng.tensor_scalar(out=nlf[:].rearrange("p a b c -> p (a b c)"),
                                                        in0=nlf[:].rearrange("p a b c -> p (a b c)"), scalar1=-1.0,
                                                        scalar2=None, op0=ALU.mult), r=[b_nlf], w=[b_nlf])
                pt = [sb("t_pt%d" % i, [128, 512], BF16, st=st) for i in range(3)]
                b_pt = [Buf() for _ in range(3)]
                srow = sb("t_srow", [128, 512], st=st)
                b_srow = Buf()
                P.op("dve", lambda eng: eng.memset(srow[:], 0.0), w=[b_srow])
                rec = sb("t_rec", [64, 512], st=st)
                b_rec = Buf()
                on = [sb("t_on%d" % i, [64, 512], BF16, st=st) for i in range(2)]
                b_on = [Buf() for _ in range(2)]
                pss = [ps("t_ps%d" % i, [128, 512], st=st) for i in range(3)]
                b_ps = [Buf() for _ in range(3)]
                po = [ps("t_po%d" % i, [128, 512], st=st) for i in range(2)]
                b_po = [Buf() for _ in range(2)]
                pr = ps("t_pr", [128, 512], st=st)
                b_pr = Buf()

                def load_head(h):
                    i = h % 2
                    P.dma("pool", lambda eng: eng.indirect_dma_start(
                        out=kt[i][0:64, 0:T2], out_offset=None, in_=KT[h].ap(),
                        in_offset=bass.IndirectOffsetOnAxis(ap=idxs[0:64, 3:4], axis=0)), b_kt[i], r=[cst], acc=[b_kt[i]])
                    P.dma("pool", lambda eng: eng.indirect_dma_start(
                        out=kt[i][0:64, T2:2 * T2], out_offset=None, in_=KT[h].ap(),
                        in_offset=bass.IndirectOffsetOnAxis(ap=idxs[0:64, 2:3], axis=0)), b_kt[i], r=[cst], acc=[b_kt[i]])
                    P.dma("pool", lambda eng: eng.indirect_dma_start(
                        out=va[i][:, 0:NT2, :].rearrange("p a b -> p (a b)"), out_offset=None, in_=VA[h].ap(),
                        in_offset=bass.IndirectOffsetOnAxis(ap=idxs[:, 1:2], axis=0)), b_va[i], r=[cst], acc=[b_va[i]])
                    P.dma("pool", lambda eng: eng.indirect_dma_start(
                        out=va[i][:, NT2:2 * NT2, :].rearrange("p a b -> p (a b)"), out_offset=None, in_=VA[h].ap(),
                        in_offset=bass.IndirectOffsetOnAxis(ap=idxs[:, 0:1], axis=0)), b_va[i], r=[cst], acc=[b_va[i]])
                    P.dma("sp", lambda eng: eng.dma_start(out=qt[i][:, :], in_=QT.ap()[h]), b_qt[i], w=[b_qt[i]])

                load_head(0)
                sc = 0
                gc = 0
                for h in range(H):
                    i = h % 2
                    if h + 1 < H:
                        load_head(h + 1)
                    for g in range(NGB):
                        oi = gc % 2
                        gc += 1
                        steps = [(0, jt) for jt in range(NT2)] + [(1, jt) for jt in range(4 * g + 4)]
                        nsteps = len(steps)
                        sis = []

                        def emit_qk(s_, blk, jt, si):
                            diag = (blk == 1 and jt >= 4 * g)
                            c0 = (jt - 4 * g) * 128 if diag else 0
                            koff = blk * T2 + jt * 128
                            P.op("pe", lambda eng, si=si, c0=c0, koff=koff, g=g, i=i, diag=diag: eng.matmul(
                                pss[si][:, c0:512], lhsT=kt[i][:, koff:koff + 128], rhs=qt[i][:, g * 512 + c0:(g + 1) * 512],
                                start=True, stop=(not diag)), r=[b_kt[i], b_aug[i], b_qt[i]], w=[b_ps[si]])
                            if diag:
                                P.op("pe", lambda eng, si=si, c0=c0: eng.matmul(
                                    pss[si][:, c0:c0 + 128], lhsT=ident_b[:], rhs=maskb[:], start=False, stop=True),
                                    r=[cst], w=[b_ps[si]])
                            P.op("act", lambda eng, si=si, c0=c0, blk=blk, jt=jt, h=h: eng.activation(
                                out=pt[si][:, c0:512], in_=pss[si][:, c0:512], func=AF.Exp, bias=nlf[:, blk, jt, h:h + 1],
                                scale=SCALE), r=[b_ps[si], b_nlf], w=[b_pt[si]])
                            return c0

                        def emit_pv(s_, blk, jt, si, c0):
                            P.op("pe", lambda eng, si=si, c0=c0, blk=blk, jt=jt, oi=oi, i=i, s_=s_, last=(s_ == nsteps - 1): eng.matmul(
                                po[oi][:, c0:512], lhsT=va[i][:, blk * NT2 + jt, :], rhs=pt[si][:, c0:512],
                                start=(s_ == 0), stop=last), r=[b_va[i], b_pt[si]], w=[b_po[oi]])

                        pend = None
                        for s_, (blk, jt) in enumerate(steps):
                            si = sc % 3
                            sc += 1
                            c0 = emit_qk(s_, blk, jt, si)
                            if pend is not None:
                                emit_pv(*pend)
                            pend = (s_, blk, jt, si, c0)
                        emit_pv(*pend)
                        P.op("act", lambda eng, oi=oi: eng.activation(out=srow[64:128, :], in_=po[oi][64:128, :], func=AF.Copy),
                             r=[b_po[oi]], w=[b_srow])
                        P.op("pe", lambda eng: eng.matmul(pr[0:64, :], lhsT=sel64[:], rhs=srow[:], start=True, stop=True),
                             r=[b_srow, cst], w=[b_pr])
                        P.op("dve", lambda eng: eng.reciprocal(out=rec[:], in_=pr[0:64, :]), r=[b_pr], w=[b_rec])
                        P.op("dve", lambda eng, oi=oi: eng.tensor_tensor(out=on[oi][:], in0=po[oi][0:64, :], in1=rec[:], op=ALU.mult),
                             r=[b_po[oi], b_rec], w=[b_on[oi]])
                        P.dma("sp", lambda eng, h=h, g=g, oi=oi: eng.dma_start(out=OT.ap()[h, :, g * 512:(g + 1) * 512], in_=on[oi][:]),
                              b_on[oi], r=[b_on[oi]])
                P.flush()

            with contextlib.ExitStack() as st:
                wo = sb("o_w", [64, H, D], BF16, st=st)
                b_w = Buf()
                P.dma("pool", lambda eng: eng.dma_start(out=wo[:], in_=attn_w_o.ap()[ja].rearrange("(h d) n -> d h n", d=64)),
                      b_w, w=[b_w])
                g1bc = sb("o_g1", [128, D], st=st)
                lng = sb("o_lng", [128, D], st=st)
                lnb = sb("o_lnb", [128, D], st=st)
                b_bc = Buf()
                pbc = ps("o_pbc", [128, 512], st=st)
                bc_from_cols(st, "o_g1", lambda k: ada[:, lj, 16 + k:17 + k], pbc, g1bc, b_bc)
                P.dma("sp", lambda eng: eng.dma_start(out=lng[:], in_=ln_g_bc.ap()[lj]), b_bc, acc=[b_bc])
                P.dma("sp", lambda eng: eng.dma_start(out=lnb[:], in_=ln_b_bc.ap()[lj]), b_bc, acc=[b_bc])
                xt = [sb("o_x%d" % i, [128, 4, D], st=st) for i in range(2)]
                b_xt = [Buf() for _ in range(2)]
                ot = [sb("o_ot%d" % i, [64, H, 128], BF16, st=st) for i in range(2)]
                b_ot = [Buf() for _ in range(2)]
                t0 = [sb("o_t0%d" % i, [128, D], st=st) for i in range(2)]
                t1 = [sb("o_t1%d" % i, [128, D], st=st) for i in range(2)]
                xo = [sb("o_xo%d" % i, [128, D], st=st) for i in range(2)]
                small = [(sb("o_st%d" % i, [128, 2, 6], st=st), sb("o_mv%d" % i, [128, 2], st=st),
                          sb("o_rs%d" % i, [128, 1], st=st)) for i in range(2)]
                b_t0 = [Buf() for _ in range(2)]
                b_t1 = [Buf() for _ in range(2)]
                b_xo = [Buf() for _ in range(2)]
                po2 = ps("o_po", [128, 1024], st=st)
                b_po2 = Buf()
                for g in range(NGB):
                    gi = g % 2
                    load_own_group(l, g, xt[gi], b_xt[gi], None)
                    for tt in range(4):
                        t = g * 4 + tt
                        i = t % 2
                        P.dma("sp", lambda eng, t=t, i=i: eng.dma_start(
                            out=ot[i][:], in_=OT.ap()[:, :, t * 128:(t + 1) * 128].rearrange("h d c -> d h c")),
                            b_ot[i], w=[b_ot[i]])
                        for hh in range(2):
                            for h in range(H):
                                P.op("pe", lambda eng, hh=hh, h=h, i=i: eng.matmul(
                                    po2[:, hh * 512:(hh + 1) * 512], lhsT=ot[i][:, h, :], rhs=wo[:, h, hh * 512:(hh + 1) * 512],
                                    start=(h == 0), stop=(h == H - 1)), r=[b_ot[i], b_w], w=[b_po2])
                        residual_ln(xt[gi][:, tt, :], [(po2[:, 0:512], 0, 512), (po2[:, 512:1024], 512, 512)], g1bc, lng, lnb,
                                    t0[i], t1[i], xo[i][:], b_xt[gi], b_po2, b_bc, b_t0[i], b_t1[i], b_xo[i], small[i])
                        P.dma("sp", lambda eng, t=t, i=i: eng.dma_start(out=xtile_dram("XA", t), in_=xo[i][:]),
                              b_xo[i], r=[b_xo[i]])
                P.flush()

        upto = cfg.upto
        conv_layer(0, "x")
        moe(0, T1, cfg.C1, 0, "XA", "XB")
        if upto >= 1:
            conv_layer(1, "XB")
            moe(1, T1, cfg.C1, 0, "XA", "X1B")
            kv_stage()
        if upto >= 2:
            attn_layer(2)
            moe(2, T2, cfg.C2, 1, "XA", "XB")
        if upto >= 3:
            attn_layer(3)
            moe(3, T2, cfg.C2, 1, "XA", "out")
        print("ninstr", P.ninstr, "ndma sems", P.ndma, flush=True)

    return nc, dbg


def _consts(cfg, hf):
    E = cfg.E
    c = {}
    c["ident_f"] = np.eye(128, dtype=np.float32)
    c["ident_b"] = np.eye(128, dtype=np.float32).astype(ml_dtypes.bfloat16)
    iu = np.arange(128)
    c["ustrict"] = (iu[:, None] < iu[None, :]).astype(np.float32)
    c["uincl"] = (iu[:, None] <= iu[None, :]).astype(np.float32)
    c["ones_f"] = np.ones((128, 128), np.float32)
    c["maskb"] = np.where(iu[None, :] >= iu[:, None], 0.0, -30000.0).astype(np.float32).astype(ml_dtypes.bfloat16)
    eb = np.zeros((128, 4, E), np.float32)
    eb[:, 0, :] = np.arange(E)[None, :] * cfg.C1 + 1
    eb[:, 1, :] = np.arange(E)[None, :] * cfg.C2 + 1
    eb[:, 2, :] = (np.arange(E)[None, :] + 1) * cfg.C1
    eb[:, 3, :] = (np.arange(E)[None, :] + 1) * cfg.C2
    c["ebase"] = eb
    selh = np.zeros((16, H, 128), np.float32)
    for h in range(H):
        selh[h, h, 64] = 1.0
        selh[h, h, 96] = 1.0
    c["selh"] = selh
    s64 = np.zeros((128, 64), np.float32)
    s64[64 + np.arange(64), np.arange(64)] = 1.0
    c["sel64"] = s64
    own, prev = (0, 2) if hf == 0 else (1, 0)
    idx = np.zeros((128, 4), np.int32)
    idx[:, 0] = own * 128 + iu
    idx[:, 1] = prev * 128 + iu
    idx[:, 2] = own * 64 + (iu % 64)
    idx[:, 3] = prev * 64 + (iu % 64)
    c["idxs"] = idx
    c["idx16"] = (own * 16 + (iu % 16)).astype(np.int32).reshape(128, 1)
    return c


def _prep_shared(inp, cfg):
    E = cfg.E
    f = lambda a: np.ascontiguousarray(np.asarray(a, dtype=np.float32))
    s = {}
    s["conv_w_in"] = f(inp["conv_w_in"])
    s["conv_wT"] = f(np.asarray(inp["conv_w"]).reshape(2, 3, NCH, 128).transpose(3, 0, 2, 1))
    s["conv_w_out"] = f(inp["conv_w_out"])
    s["kv_ada_w"] = f(inp["kv_ada_w"])
    s["kv_ada_bT"] = f(np.asarray(inp["kv_ada_b"]).reshape(16, 128).T)
    s["w_kvf"] = f(inp["w_kvf"])
    s["b_f_bc"] = f(np.broadcast_to(np.asarray(inp["b_f"])[None, :], (128, H)))
    s["attn_w_q"] = f(inp["attn_w_q"])
    s["attn_w_o"] = f(inp["attn_w_o"])
    s["ada_w"] = f(np.asarray(inp["ada_w"]).reshape(8, D, 3 * D))
    s["ada_bT"] = f(np.asarray(inp["ada_b"]).reshape(8, 24, 128).transpose(2, 0, 1))
    s["ln_g_bc"] = f(np.broadcast_to(np.asarray(inp["ln_g"]).reshape(8, 1, D), (8, 128, D)))
    s["ln_b_bc"] = f(np.broadcast_to(np.asarray(inp["ln_b"]).reshape(8, 1, D), (8, 128, D)))
    s["router_w"] = f(inp["router_w"])
    s["router_b_bc"] = f(np.broadcast_to(np.asarray(inp["router_b"])[:, None, :], (4, 128, E)))
    s["exp_w_gu"] = f(inp["exp_w_gu"])
    s["exp_b_guT"] = f(np.asarray(inp["exp_b_gu"]).reshape(4, E, 16, 128).transpose(0, 3, 1, 2))
    s["exp_w_down"] = f(inp["exp_w_down"])
    s["exp_b_down"] = f(inp["exp_b_down"])
    return s


def run(inputs, cfg, debug=False):
    x = np.asarray(inputs["x"], dtype=np.float32)
    c = np.asarray(inputs["c"], dtype=np.float32)
    Bn = x.shape[0]
    ncores = 2 * Bn
    nc, dbg = build_program(cfg, debug=debug)
    shared = _prep_shared(inputs, cfg)
    in_maps = []
    for core in range(ncores):
        b, hf = core // 2, core % 2
        m = dict(shared)
        m["x"] = np.ascontiguousarray(x[b])
        m["cT"] = np.ascontiguousarray(c[b].reshape(NCH, 128).T)
        m.update(_consts(cfg, hf))
        in_maps.append(m)
    res = run_bass_kernel_spmd(nc, in_maps, core_ids=list(range(ncores)))
    return res, dbg


def kernel(**inputs):
    x = np.asarray(inputs["x"])
    Bn, S, _ = x.shape
    E = np.asarray(inputs["router_w"]).shape[-1]
    cfg = Cfg(S, E)
    res, _ = run(inputs, cfg)
    out = np.zeros((Bn, S, D), np.float32)
    for core in range(2 * Bn):
        b, hf = core // 2, core % 2
        out[b, hf * cfg.T2:(hf + 1) * cfg.T2] = res.results[core]["out"]
    return out
```

```python
import contextlib
import numpy as np
import ml_dtypes
import concourse.bass as bass
import concourse.mybir as mybir
from concourse.bass_utils import run_bass_kernel_spmd

F32 = mybir.dt.float32
BF16 = mybir.dt.bfloat16
I32 = mybir.dt.int32
ALU = mybir.AluOpType
AF = mybir.ActivationFunctionType
AX = mybir.AxisListType

D = 1024
NCH = 8
H = 16
DH = 64
TOPK = 4
ALPHA = (2.0 * 4) ** 0.25
LN_EPS = 1e-5
LIMIT = 7.0
SW_ALPHA = 1.702
SCALE = DH ** -0.5
ROWW = 1024 + 64

SELF_WAIT = True
ENGS = ["pe", "act", "dve", "pool", "sp"]
BLK = {"pe": "tensor", "act": "scalar", "dve": "vector", "pool": "gpsimd", "sp": "sync"}


class Buf:
    __slots__ = ("w", "r", "sem", "cnt", "name")

    def __init__(self, name=""):
        self.w = {}
        self.r = {}
        self.sem = None
        self.cnt = 0
        self.name = name


class Prog:
    def __init__(self, nc, stack):
        self.nc = nc
        self.stack = stack
        self.sems = {}
        self.ecnt = {}
        for e in ["pe", "act", "dve", "pool"]:
            self.sems[("e", e)] = stack.enter_context(nc.semaphore("s_" + e))
            self.ecnt[e] = 0
        self.ndma = 0
        self.dma_bufs = []
        self.free_sems = []
        self.q = {e: [] for e in ENGS}
        self.waited = {e: {} for e in ENGS}
        self.ninstr = 0

    def _deps(self, e, r, w, acc):
        deps = {}
        for b in r:
            for k, v in b.w.items():
                if deps.get(k, 0) < v:
                    deps[k] = v
        for b in w:
            for dd in (b.w, b.r):
                for k, v in dd.items():
                    if deps.get(k, 0) < v:
                        deps[k] = v
        for b in acc:
            for k, v in b.r.items():
                if deps.get(k, 0) < v:
                    deps[k] = v
        wd = self.waited[e]
        for k, v in deps.items():
            if k == ("e", e) and (e == "pe" or (not SELF_WAIT and e in ("act", "dve"))):
                continue
            if wd.get(k, 0) < v:
                wd[k] = v
                sem = self.sems[k]
                self.q[e].append(lambda eng, sem=sem, v=v: eng.wait_ge(sem, v))
                self.ninstr += 1

    def _mark(self, key, val, r, w, acc):
        for b in r:
            if b.r.get(key, 0) < val:
                b.r[key] = val
        for b in w:
            b.w = {key: val}
            b.r = {}
        for b in acc:
            if b.w.get(key, 0) < val:
                b.w[key] = val

    def op(self, e, fn, r=(), w=(), acc=()):
        self._deps(e, r, w, acc)
        self.ecnt[e] += 1
        key = ("e", e)
        sem = self.sems[key]
        self.q[e].append(lambda eng, fn=fn, sem=sem: fn(eng).then_inc(sem, 1))
        self.ninstr += 1
        self._mark(key, self.ecnt[e], r, w, acc)

    def dma(self, e, fn, sb, r=(), w=(), acc=()):
        self._deps(e, r, w, acc)
        if sb.sem is None:
            if self.free_sems:
                sb.sem, sb.cnt = self.free_sems.pop()
            else:
                sb.sem = ("d", self.ndma)
                sb.cnt = 0
                self.sems[sb.sem] = self.stack.enter_context(self.nc.semaphore("d%d" % self.ndma))
                self.ndma += 1
            self.dma_bufs.append(sb)
        sb.cnt += 1
        key = sb.sem
        sem = self.sems[key]
        self.q[e].append(lambda eng, fn=fn, sem=sem: fn(eng).then_inc(sem, 16))
        self.ninstr += 1
        self._mark(key, 16 * sb.cnt, r, w, acc)

    def flush(self):
        wd = self.waited["sp"]
        for sb in self.dma_bufs:
            if sb.cnt and wd.get(sb.sem, 0) < 16 * sb.cnt:
                wd[sb.sem] = 16 * sb.cnt
                sem = self.sems[sb.sem]
                self.q["sp"].append(lambda eng, sem=sem, v=16 * sb.cnt: eng.wait_ge(sem, v))
        with self.nc.Block() as block:
            for e in ENGS:
                if self.q[e]:
                    L = self.q[e]

                    def body(eng, L=L):
                        for f in L:
                            f(eng)

                    getattr(block, BLK[e])(body)
        self.q = {e: [] for e in ENGS}
        full = {}
        for e in ["pe", "act", "dve", "pool"]:
            full[("e", e)] = self.ecnt[e]
        for sb in self.dma_bufs:
            full[sb.sem] = 16 * sb.cnt
        for e in ENGS:
            self.waited[e].update(full)
        for sb in self.dma_bufs:
            self.free_sems.append((sb.sem, sb.cnt))
            sb.sem = None
        self.dma_bufs = []


class Cfg:
    def __init__(self, T1, E, cap_mult=1.375):
        self.T1 = T1
        self.T2 = T1 // 2
        self.E = E
        self.NT1 = T1 // 128
        self.NT2 = self.T2 // 128

        def cap(T):
            mean = T * TOPK / E
            c = int(np.ceil(mean * cap_mult / 128.0)) * 128
            return max(c, 128)

        self.C1 = cap(self.T1)
        cap_mult = max(cap_mult, 1.5)
        self.C2 = cap(self.T2)
        self.upto = 3


def build_program(cfg, debug=False):
    T1, T2, E = cfg.T1, cfg.T2, cfg.E
    NT1, NT2 = cfg.NT1, cfg.NT2
    nc = bass.Bass("TRN2", target_bir_lowering=False)

    def din(name, shape, dt=F32):
        return nc.dram_tensor(name, list(shape), dt, kind="ExternalInput")

    x_in = din("x", [T1, D])
    cT_in = din("cT", [128, NCH])
    conv_w_in = din("conv_w_in", [2, D, 3 * D])
    conv_wT = din("conv_wT", [128, 2, NCH, 3])
    conv_w_out = din("conv_w_out", [2, D, D])
    kv_ada_w = din("kv_ada_w", [D, 2 * D])
    kv_ada_bT = din("kv_ada_bT", [128, 16])
    w_kvf = din("w_kvf", [D, 2 * D + H])
    b_f_bc = din("b_f_bc", [128, H])
    attn_w_q = din("attn_w_q", [2, D, D])
    attn_w_o = din("attn_w_o", [2, D, D])
    ada_w = din("ada_w", [8, D, 3 * D])
    ada_bT = din("ada_bT", [128, 8, 24])
    ln_g_bc = din("ln_g_bc", [8, 128, D])
    ln_b_bc = din("ln_b_bc", [8, 128, D])
    router_w = din("router_w", [4, D, E])
    router_b_bc = din("router_b_bc", [4, 128, E])
    exp_w_gu = din("exp_w_gu", [4, E, D, 2 * D])
    exp_b_guT = din("exp_b_guT", [4, 128, E, 16])
    exp_w_down = din("exp_w_down", [4, E, D, D])
    exp_b_down = din("exp_b_down", [4, E, D])
    ident_f_in = din("ident_f", [128, 128])
    ident_b_in = din("ident_b", [128, 128], BF16)
    ustrict_in = din("ustrict", [128, 128])
    uincl_in = din("uincl", [128, 128])
    ones_in = din("ones_f", [128, 128])
    maskb_in = din("maskb", [128, 128], BF16)
    ebase_in = din("ebase", [128, 4, E])
    selh_in = din("selh", [16, H, 128])
    sel64_in = din("sel64", [128, 64])
    idx_in = din("idxs", [128, 4], I32)
    idx16_in = din("idx16", [128, 1], I32)

    out_t = nc.dram_tensor("out", [T2, D], F32, kind="ExternalOutput")

    def dscr(name, shape, dt=F32):
        if debug and name in debug:
            return nc.dram_tensor(name, list(shape), dt, kind="ExternalOutput")
        return nc.dram_tensor(name, list(shape), dt)

    XA = dscr("XA", [T1, D])
    XB = dscr("XB", [T1, D])
    NG1 = NT1 // 4
    NGB = NT2 // 4
    X1B = [nc.dram_tensor("X1B%d" % g, [2 * 128, 4 * D], F32) for g in range(NGB)]
    XS = dscr("XS", [E * cfg.C1, ROWW], BF16)
    YS = dscr("YS", [E * cfg.C1, D], F32)
    SIDX = dscr("SIDX", [128, NT1 * 4], I32)
    KT = [nc.dram_tensor("KT%d" % h, [3 * 64, T2], BF16) for h in range(H)]
    VA = [nc.dram_tensor("VA%d" % h, [3 * 128, NT2 * 128], BF16) for h in range(H)]
    LF = nc.dram_tensor("LF", [3 * 128, NT2 * H], F32)
    LFT = nc.dram_tensor("LFT", [3 * 16, T2], F32)
    QT = nc.dram_tensor("QT", [H, 128, T2], BF16)
    OT = nc.dram_tensor("OT", [H, 64, T2], BF16)
    dbg = {}

    stack = contextlib.ExitStack()
    with stack:
        P = Prog(nc, stack)

        uniq = [0]

        def sb(name, shape, dt=F32, st=stack):
            uniq[0] += 1
            return st.enter_context(nc.sbuf_tensor("%s_s%d" % (name, uniq[0]), list(shape), dt))

        def ps(name, shape, dt=F32, st=stack):
            uniq[0] += 1
            return st.enter_context(nc.psum_tensor("%s_p%d" % (name, uniq[0]), list(shape), dt))

        ident_f = sb("ident_f", [128, 128])
        ident_b = sb("ident_b", [128, 128], BF16)
        ustrict = sb("ustrict", [128, 128])
        uincl = sb("uincl", [128, 128])
        ones_f = sb("ones_f", [128, 128])
        maskb = sb("maskb", [128, 128], BF16)
        ebase = sb("ebase", [128, 4, E])
        selh = sb("selh", [16, H, 128])
        sel64 = sb("sel64", [128, 64])
        idxs = sb("idxs", [128, 4], I32)
        idx16 = sb("idx16", [128, 1], I32)
        ada = sb("ada", [128, 8, 24])
        kvada = sb("kvada", [128, 16])
        convw = sb("convw", [128, 2, NCH, 3])
        bfb = sb("bfb", [128, H])
        sidx = sb("sidx", [128, NT1, 4], I32)
        cst = Buf("cst")

        for (t, src) in [(ident_f, ident_f_in), (ident_b, ident_b_in), (ustrict, ustrict_in), (uincl, uincl_in),
                         (ones_f, ones_in), (maskb, maskb_in), (ebase, ebase_in), (selh, selh_in),
                         (sel64, sel64_in), (idxs, idx_in), (idx16, idx16_in), (convw, conv_wT), (bfb, b_f_bc)]:
            P.dma("sp", lambda eng, t=t, src=src: eng.dma_start(out=t[:], in_=src.ap()), cst, w=[cst])

        with contextlib.ExitStack() as st:
            cT = sb("cT", [128, NCH], st=st)
            cond = sb("cond", [128, NCH], st=st)
            adab = sb("adab", [128, 8, 24], st=st)
            kvb = sb("kvb", [128, 16], st=st)
            wt = [sb("adaw%d" % i, [128, NCH, 512], st=st) for i in range(2)]
            wtb = [Buf() for _ in range(2)]
            pacc = ps("pacc", [128, 512], st=st)
            b_c, b_cond, b_ps, b_ada = Buf(), Buf(), Buf(), Buf()
            P.dma("sp", lambda eng: eng.dma_start(out=cT[:], in_=cT_in.ap()), b_c, w=[b_c])
            P.dma("sp", lambda eng: eng.dma_start(out=adab[:], in_=ada_bT.ap()), b_c, w=[b_c])
            P.dma("sp", lambda eng: eng.dma_start(out=kvb[:], in_=kv_ada_bT.ap()), b_c, w=[b_c])
            P.op("act", lambda eng: eng.activation(out=cond[:], in_=cT[:], func=AF.Silu), r=[b_c], w=[b_cond])
            it = 0
            jobs = [(lj, ada_w.ap()[lj], 6, lj) for lj in range(8)] + [(8, kv_ada_w.ap(), 4, None)]
            for (lj, wsrc, ncb, _) in jobs:
                for cb in range(ncb):
                    i = it % 2
                    it += 1
                    src = wsrc[:, cb * 512:(cb + 1) * 512].rearrange("(k p) n -> p k n", p=128)
                    P.dma("sp", lambda eng, i=i, src=src: eng.dma_start(out=wt[i][:], in_=src), wtb[i], w=[wtb[i]])
                    for m in range(4):
                        col = cb * 4 + m
                        for k in range(NCH):
                            P.op("pe", lambda eng, i=i, m=m, k=k, col=col: eng.matmul(
                                pacc[:, col:col + 1], lhsT=wt[i][:, k, m * 128:(m + 1) * 128], rhs=cond[:, k:k + 1],
                                start=(k == 0), stop=(k == NCH - 1)), r=[wtb[i], b_cond], w=[b_ps])
                ncol = ncb * 4
                if lj < 8:
                    P.op("dve", lambda eng, lj=lj: eng.tensor_tensor(out=ada[:, lj, :], in0=pacc[:, 0:24], in1=adab[:, lj, :],
                                                                  op=ALU.add), r=[b_ps, b_c], w=[b_ada])
                    P.op("dve", lambda eng, lj=lj: eng.tensor_scalar(out=ada[:, lj, 8:24], in0=ada[:, lj, 8:24], scalar1=1.0,
                                                                  scalar2=None, op0=ALU.add), r=[b_ada], w=[b_ada])
                else:
                    P.op("dve", lambda eng: eng.tensor_tensor(out=kvada[:], in0=pacc[:, 0:16], in1=kvb[:], op=ALU.add),
                         r=[b_ps, b_c], w=[b_ada])
                    P.op("dve", lambda eng: eng.tensor_scalar(out=kvada[:, 8:16], in0=kvada[:, 8:16], scalar1=1.0,
                                                           scalar2=None, op0=ALU.add), r=[b_ada], w=[b_ada])
            P.flush()

        def bc_from_cols(st, name, cols_ap_fn, pbank, out_tile, deps_b):
            diag = sb(name + "_dg", [128, 128], st=st)
            bd, bp = Buf(), Buf()
            for k in range(NCH):
                P.op("dve", lambda eng, k=k: eng.tensor_scalar(out=diag[:], in0=ident_f[:], scalar1=cols_ap_fn(k),
                                                             scalar2=None, op0=ALU.mult), r=[cst], w=[bd])
                P.op("pe", lambda eng: eng.matmul(pbank[:, 0:128], lhsT=ones_f[:], rhs=diag[:], start=True, stop=True),
                     r=[bd, cst], w=[bp])
                P.op("act", lambda eng, k=k: eng.activation(out=out_tile[:, k * 128:(k + 1) * 128], in_=pbank[:, 0:128],
                                                          func=AF.Copy), r=[bp], w=[deps_b])

        def load_w_bf16(tile3, src2d, b, eng_name="pool"):
            src = src2d.rearrange("(k p) n -> p k n", p=128)
            P.dma(eng_name, lambda eng: eng.dma_start(out=tile3, in_=src), b, w=[b])

        def residual_ln(xt, sub_ps_list, g1bc, lngbc, lnbbc, t0, t1, outt, b_x, b_sub, b_bc, b_t0, b_t1, b_out, st_small):
            stats, mv, rstd = st_small
            for (pap, c0, n) in sub_ps_list:
                P.op("dve", lambda eng, pap=pap, c0=c0, n=n: eng.tensor_tensor(out=t0[:, c0:c0 + n], in0=pap, in1=g1bc[:, c0:c0 + n],
                                                                             op=ALU.mult), r=[b_sub, b_bc], w=[b_t0])
            P.op("dve", lambda eng: eng.scalar_tensor_tensor(out=t1[:], in0=xt, scalar=ALPHA, in1=t0[:], op0=ALU.mult,
                                                           op1=ALU.add), r=[b_x, b_t0], w=[b_t1])
            for c in range(2):
                P.op("dve", lambda eng, c=c: eng.bn_stats(out=stats[:, c, :], in_=t1[:, c * 512:(c + 1) * 512]), r=[b_t1], w=[b_t0])
            P.op("dve", lambda eng: eng.bn_aggr(out=mv[:], in_=stats[:].rearrange("p a b -> p (a b)")), r=[b_t0], w=[b_t0])
            P.op("act", lambda eng: eng.activation(out=rstd[:], in_=mv[:, 1:2], func=AF.Sqrt, bias=epsc[:, 0:1], scale=1.0),
                 r=[b_t0, cst], w=[b_t0])
            P.op("dve", lambda eng: eng.reciprocal(out=rstd[:], in_=rstd[:]), r=[b_t0], w=[b_t0])
            P.op("dve", lambda eng: eng.tensor_scalar(out=mv[:, 0:1], in0=mv[:, 0:1], scalar1=rstd[:, 0:1], scalar2=-1.0,
                                                    op0=ALU.mult, op1=ALU.mult), r=[b_t0], w=[b_t0])
            P.op("act", lambda eng: eng.activation(out=t1[:], in_=t1[:], func=AF.Identity, bias=mv[:, 0:1], scale=rstd[:, 0:1]),
                 r=[b_t0, b_t1], w=[b_t1])
            P.op("dve", lambda eng: eng.tensor_tensor(out=t1[:], in0=t1[:], in1=lngbc[:], op=ALU.mult), r=[b_t1, b_bc], w=[b_t1])
            P.op("dve", lambda eng: eng.tensor_tensor(out=outt, in0=t1[:], in1=lnbbc[:], op=ALU.add), r=[b_t1, b_bc], w=[b_out])

        epsc = sb("epsc", [128, 1])
        P.op("dve", lambda eng: eng.memset(epsc[:], LN_EPS), w=[cst])

        def xtile_dram(kind, t):
            if kind == "x":
                return x_in.ap()[t * 128:(t + 1) * 128, :]
            if kind == "XA":
                return XA.ap()[t * 128:(t + 1) * 128, :]
            if kind == "XB":
                return XB.ap()[t * 128:(t + 1) * 128, :]
            if kind == "out":
                return out_t.ap()[t * 128:(t + 1) * 128, :]
            if kind == "X1B":
                B, j = t // NT2, t % NT2
                return X1B[j // 4].ap()[B * 128:(B + 1) * 128, (j % 4) * D:(j % 4 + 1) * D]
            raise ValueError(kind)

        def load_x(kind, t, tile_ap, b, eng="sp"):
            if kind == "X1Bown":
                g, jj = t // 4, t % 4
                src = X1B[g].ap()[:, jj * D:(jj + 1) * D]
                raise ValueError("use load_x_group")
            P.dma(eng, lambda e_: e_.dma_start(out=tile_ap, in_=xtile_dram(kind, t)), b, w=[b])

        def moe(l, T, C, ci, src_kind, dst_kind):
            NT = T // 128
            lj = 2 * l + 1
            NS = C // 128
            XSall, YSall = Buf("XS"), Buf("YS")
            with contextlib.ExitStack() as st:
                rw = sb("rw", [128, NCH, E], st=st)
                rbb = sb("rbb", [128, E], st=st)
                b_rw = Buf()
                P.dma("sp", lambda eng: eng.dma_start(out=rw[:], in_=router_w.ap()[l].rearrange("(k p) e -> p k e", p=128)),
                      b_rw, w=[b_rw])
                P.dma("sp", lambda eng: eng.dma_start(out=rbb[:], in_=router_b_bc.ap()[l]), b_rw, w=[b_rw])
                NB = 3
                xt = [sb("r_x%d" % i, [128, D], st=st) for i in range(NB)]
                b_xt = [Buf() for _ in range(NB)]
                hT = [sb("r_hT%d" % i, [128, NCH, 128], st=st) for i in range(2)]
                b_hT = [Buf() for _ in range(2)]
                row = [sb("r_row%d" % i, [128, ROWW], BF16, st=st) for i in range(NB)]
                b_row = [Buf() for _ in range(NB)]
                ptr = [ps("r_ptr%d" % i, [128, 512], st=st) for i in range(2)]
                b_ptr = [Buf() for _ in range(2)]
                plg = ps("r_plg", [128, 512], st=st)
                b_plg = Buf()
                prk = ps("r_prk", [128, 512], st=st)
                b_prk = Buf()
                lg = sb("r_lg", [128, E], st=st)
                v8 = sb("r_v8", [128, 8], st=st)
                msk = sb("r_msk", [128, E], st=st)
                msum = sb("r_msum", [128, E], st=st)
                ex = sb("r_ex", [128, E], st=st)
                ssum = sb("r_ssum", [128, 1], st=st)
                nmx = sb("r_nmx", [128, 1], st=st)
                sv = sb("r_sv", [128, E], st=st)
                sv8 = sb("r_sv8", [128, 8], st=st)
                b_s = Buf()
                b_msum = Buf()
                b_sidx = Buf()
                P.op("dve", lambda eng: eng.memset(msum[:], 0.0), w=[b_msum])
                for t in range(NT):
                    i3, i2 = t % NB, t % 2
                    load_x(src_kind, t, xt[i3][:], b_xt[i3])
                    for half in range(2):
                        pb = ptr[half]
                        for kk in range(4):
                            k = half * 4 + kk
                            P.op("pe", lambda eng, pb=pb, kk=kk, k=k, i3=i3: eng.transpose(
                                pb[:, kk * 128:(kk + 1) * 128], xt[i3][:, k * 128:(k + 1) * 128], ident_f[:]),
                                r=[b_xt[i3], cst], w=[b_ptr[half]])
                        for kk in range(4):
                            k = half * 4 + kk
                            P.op("act", lambda eng, pb=pb, kk=kk, k=k, i2=i2: eng.activation(
                                out=hT[i2][:, k, :], in_=pb[:, kk * 128:(kk + 1) * 128], func=AF.Identity,
                                bias=ada[:, lj, k:k + 1], scale=ada[:, lj, 8 + k:9 + k]), r=[b_ptr[half]], w=[b_hT[i2]])
                    for k in range(NCH):
                        P.op("pe", lambda eng, k=k, i2=i2: eng.matmul(plg[:, 0:E], lhsT=hT[i2][:, k, :], rhs=rw[:, k, :],
                                                                    start=(k == 0), stop=(k == NCH - 1)),
                             r=[b_hT[i2], b_rw], w=[b_plg])
                    P.op("dve", lambda eng: eng.tensor_tensor(out=lg[:], in0=plg[:, 0:E], in1=rbb[:], op=ALU.add),
                         r=[b_plg, b_rw], w=[b_s])
                    P.op("dve", lambda eng: eng.max(out=v8[:], in_=lg[:]), r=[b_s], w=[b_s])
                    P.op("dve", lambda eng: eng.tensor_scalar(out=msk[:], in0=lg[:], scalar1=v8[:, 3:4], scalar2=None,
                                                            op0=ALU.is_ge), r=[b_s], w=[b_s])
                    P.op("dve", lambda eng: eng.tensor_scalar(out=nmx[:], in0=v8[:, 0:1], scalar1=-1.0, scalar2=None,
                                                            op0=ALU.mult), r=[b_s], w=[b_s])
                    P.op("act", lambda eng: eng.activation(out=ex[:], in_=lg[:], func=AF.Exp, bias=nmx[:, 0:1], scale=1.0),
                         r=[b_s], w=[b_s])
                    P.op("dve", lambda eng: eng.tensor_tensor(out=ex[:], in0=ex[:], in1=msk[:], op=ALU.mult), r=[b_s], w=[b_s])
                    P.op("dve", lambda eng: eng.tensor_reduce(out=ssum[:], in_=ex[:], axis=AX.X, op=ALU.add), r=[b_s], w=[b_s])
                    P.op("dve", lambda eng: eng.reciprocal(out=ssum[:], in_=ssum[:]), r=[b_s], w=[b_s])
                    P.op("dve", lambda eng, i3=i3: eng.tensor_scalar(
                        out=row[i3][:, 1024:ROWW].bitcast(F32)[:, 0:E], in0=ex[:], scalar1=ssum[:, 0:1], scalar2=None, op0=ALU.mult),
                        r=[b_s], w=[b_row[i3]])
                    P.op("pool", lambda eng, i3=i3: eng.tensor_copy(out=row[i3][:, 0:1024], in_=xt[i3][:]),
                         r=[b_xt[i3]], w=[b_row[i3]])
                    P.op("pe", lambda eng: eng.matmul(prk[:, 0:E], lhsT=ustrict[:], rhs=msk[:], start=True, stop=False),
                         r=[b_s, cst], w=[b_prk])
                    P.op("pe", lambda eng: eng.matmul(prk[:, 0:E], lhsT=ones_f[:], rhs=msum[:], start=False, stop=True),
                         r=[b_msum, cst], w=[b_prk])
                    P.op("dve", lambda eng: eng.tensor_tensor(out=sv[:], in0=prk[:, 0:E], in1=ebase[:, ci, :], op=ALU.add),
                         r=[b_prk, cst], w=[b_s])
                    P.op("dve", lambda eng: eng.tensor_tensor(out=sv[:], in0=sv[:], in1=ebase[:, 2 + ci, :], op=ALU.min),
                         r=[b_s, cst], w=[b_s])
                    P.op("dve", lambda eng: eng.tensor_tensor(out=sv[:], in0=sv[:], in1=msk[:], op=ALU.mult), r=[b_s], w=[b_s])
                    P.op("dve", lambda eng: eng.tensor_tensor(out=msum[:], in0=msum[:], in1=msk[:], op=ALU.add),
                         r=[b_s, b_msum], w=[b_msum])
                    P.op("dve", lambda eng: eng.max(out=sv8[:], in_=sv[:]), r=[b_s], w=[b_s])
                    P.op("dve", lambda eng: eng.tensor_scalar(out=sv8[:, 0:4], in0=sv8[:, 0:4], scalar1=-1.0, scalar2=None,
                                                            op0=ALU.add), r=[b_s], w=[b_s])
                    P.op("dve", lambda eng, t=t: eng.tensor_copy(out=sidx[:, t, :], in_=sv8[:, 0:4]), r=[b_s], w=[b_sidx])
                    for kk in range(TOPK):
                        P.dma("pool", lambda eng, t=t, kk=kk, i3=i3: eng.indirect_dma_start(
                            out=XS.ap(), out_offset=bass.IndirectOffsetOnAxis(ap=sidx[:, t, kk:kk + 1], axis=0),
                            in_=row[i3][:], in_offset=None),
                            b_row[i3], r=[b_row[i3], b_sidx], acc=[XSall])
                if debug and "SIDX" in debug and l == 0:
                    P.dma("sp", lambda eng: eng.dma_start(out=SIDX.ap(), in_=sidx[:].rearrange("p a b -> p (a b)")), b_sidx, r=[b_sidx])
                P.flush()

            with contextlib.ExitStack() as st:
                NSB = (C + 511) // 512
                wgu = [sb("b_wgu%d" % i, [128, NCH, 2 * D], BF16, st=st) for i in range(2)]
                wdn = [sb("b_wdn%d" % i, [128, NCH, D], BF16, st=st) for i in range(2)]
                bdn = [sb("b_bdn%d" % i, [1, D], BF16, st=st) for i in range(2)]
                b_w = [Buf() for _ in range(2)]
                bgu = sb("b_bgu", [128, E, 16], st=st)
                b_bgu = Buf()
                P.dma("sp", lambda eng: eng.dma_start(out=bgu[:], in_=exp_b_guT.ap()[l]), b_bgu, w=[b_bgu])
                P.op("dve", lambda eng: eng.tensor_scalar(out=bgu[:, :, 8:16], in0=bgu[:, :, 8:16], scalar1=1.0, scalar2=None,
                                                        op0=ALU.add), r=[b_bgu], w=[b_bgu])
                xr = [sb("b_xr%d" % i, [128, 4, ROWW], BF16, st=st) for i in range(3)]
                b_xr = [Buf() for _ in range(3)]
                xT = [sb("b_xT%d" % i, [128, NCH, 512], BF16, st=st) for i in range(2)]
                b_xT = [Buf() for _ in range(2)]
                aT = [sb("b_aT%d" % i, [128, NCH, 512], BF16, st=st) for i in range(2)]
                b_aT = [Buf() for _ in range(2)]
                gs = [sb("b_g%d" % i, [128, 512], st=st) for i in range(3)]
                sg = [sb("b_sg%d" % i, [128, 512], st=st) for i in range(3)]
                us = [sb("b_u%d" % i, [128, 512], st=st) for i in range(3)]
                b_g = [Buf() for _ in range(3)]
                b_sg = [Buf() for _ in range(3)]
                b_u = [Buf() for _ in range(3)]
                yo = [sb("b_y%d" % i, [128, D], st=st) for i in range(2)]
                b_yo = [Buf() for _ in range(2)]
                onesb = sb("b_ones", [1, 128], BF16, st=st)
                P.op("dve", lambda eng: eng.memset(onesb[:], 1.0), w=[cst])
                ptr = [ps("b_ptr%d" % i, [128, 1024], BF16, st=st) for i in range(1)]
                b_ptr = [Buf() for _ in range(1)]
                pg = [ps("b_pg%d" % i, [128, 512], st=st) for i in range(2)]
                pu = [ps("b_pu%d" % i, [128, 512], st=st) for i in range(2)]
                b_pg = [Buf() for _ in range(2)]
                b_pu = [Buf() for _ in range(2)]
                py = [ps("b_py%d" % i, [128, 512], st=st) for i in range(3)]
                b_py = [Buf() for _ in range(3)]

                def load_expert(e):
                    i = e % 2
                    load_w_bf16(wgu[i][:], exp_w_gu.ap()[l, e], b_w[i])
                    load_w_bf16(wdn[i][:], exp_w_down.ap()[l, e], b_w[i])
                    P.dma("pool", lambda eng: eng.dma_start(out=bdn[i][:], in_=exp_b_down.ap()[l, e:e + 1, :]), b_w[i], acc=[b_w[i]])

                blocks = []
                for e in range(E):
                    for sbk in range(NSB):
                        blocks.append((e, sbk, len(blocks) % 2, len(blocks) % 3))
                state = {"y": 0, "py": 0}

                def emit_T(blk):
                    e, sbk, j, j3 = blk
                    n = min(512, C - sbk * 512)
                    nt = n // 128
                    s0 = e * C + sbk * 512
                    P.dma("sp", lambda eng: eng.dma_start(
                        out=xr[j3][:, 0:nt, :], in_=XS.ap()[s0:s0 + n, :].rearrange("(t p) c -> p t c", p=128)),
                        b_xr[j3], r=[XSall], w=[b_xr[j3]])
                    for k in range(NCH):
                        for tt in range(nt):
                            P.op("pe", lambda eng, k=k, tt=tt: eng.transpose(
                                ptr[0][:, tt * 128:(tt + 1) * 128], xr[j3][:, tt, k * 128:(k + 1) * 128], ident_b[:]),
                                r=[b_xr[j3], cst], w=[b_ptr[0]])
                        P.op("act", lambda eng, k=k: eng.activation(
                            out=xT[j][:, k, 0:n], in_=ptr[0][:, 0:n], func=AF.Identity, bias=ada[:, lj, k:k + 1],
                            scale=ada[:, lj, 8 + k:9 + k]), r=[b_ptr[0]], w=[b_xT[j]])

                def emit_GU(blk):
                    e, sbk, j, j3 = blk
                    i = e % 2
                    n = min(512, C - sbk * 512)
                    pend = None
                    for f in range(NCH):
                        q2 = f % 2
                        q3 = f % 3
                        for k in range(NCH):
                            P.op("pe", lambda eng, f=f, k=k, q2=q2: eng.matmul(
                                pg[q2][:, 0:n], lhsT=wgu[i][:, k, f * 128:(f + 1) * 128], rhs=xT[j][:, k, 0:n],
                                start=(k == 0), stop=(k == NCH - 1)), r=[b_w[i], b_xT[j]], w=[b_pg[q2]])
                        for k in range(NCH):
                            P.op("pe", lambda eng, f=f, k=k, q2=q2: eng.matmul(
                                pu[q2][:, 0:n], lhsT=wgu[i][:, k, D + f * 128:D + (f + 1) * 128], rhs=xT[j][:, k, 0:n],
                                start=(k == 0), stop=(k == NCH - 1)), r=[b_w[i], b_xT[j]], w=[b_pu[q2]])
                        P.op("dve", lambda eng, f=f, q2=q2, q3=q3: eng.tensor_scalar(
                            out=gs[q3][:, 0:n], in0=pg[q2][:, 0:n], scalar1=bgu[:, e, f:f + 1], scalar2=LIMIT,
                            op0=ALU.add, op1=ALU.min), r=[b_pg[q2], b_bgu], w=[b_g[q3]])
                        P.op("act", lambda eng, q3=q3: eng.activation(out=sg[q3][:, 0:n], in_=gs[q3][:, 0:n],
                                                                     func=AF.Sigmoid, scale=SW_ALPHA),
                             r=[b_g[q3]], w=[b_sg[q3]])
                        P.op("dve", lambda eng, f=f, q2=q2, q3=q3: eng.tensor_scalar(
                            out=us[q3][:, 0:n], in0=pu[q2][:, 0:n], scalar1=bgu[:, e, 8 + f:9 + f], scalar2=LIMIT + 1.0,
                            op0=ALU.add, op1=ALU.min), r=[b_pu[q2], b_bgu], w=[b_u[q3]])
                        P.op("pool", lambda eng, q3=q3: eng.tensor_tensor(out=sg[q3][:, 0:n], in0=gs[q3][:, 0:n],
                                                                         in1=sg[q3][:, 0:n], op=ALU.mult),
                             r=[b_g[q3], b_sg[q3]], w=[b_sg[q3]])
                        if pend is not None:
                            pend()

                        def fin(f=f, q3=q3):
                            P.op("dve", lambda eng: eng.scalar_tensor_tensor(
                                out=aT[j][:, f, 0:n], in0=us[q3][:, 0:n], scalar=1.0 - LIMIT, in1=sg[q3][:, 0:n],
                                op0=ALU.max, op1=ALU.mult), r=[b_u[q3], b_sg[q3]], w=[b_aT[j]])
                        pend = fin
                    pend()

                def emit_D(blk):
                    e, sbk, j, j3 = blk
                    i = e % 2
                    n = min(512, C - sbk * 512)
                    nt = n // 128
                    s0 = e * C + sbk * 512
                    for tt in range(nt):
                        y2 = state["y"] % 2
                        state["y"] += 1
                        for hh in range(2):
                            pi = state["py"] % 3
                            state["py"] += 1
                            for f in range(NCH):
                                P.op("pe", lambda eng, tt=tt, hh=hh, f=f, pi=pi: eng.matmul(
                                    py[pi][:, :], lhsT=aT[j][:, f, tt * 128:(tt + 1) * 128],
                                    rhs=wdn[i][:, f, hh * 512:(hh + 1) * 512], start=(f == 0), stop=False),
                                    r=[b_aT[j], b_w[i]], w=[b_py[pi]])
                            P.op("pe", lambda eng, hh=hh, pi=pi: eng.matmul(
                                py[pi][:, :], lhsT=onesb[:], rhs=bdn[i][:, hh * 512:(hh + 1) * 512],
                                start=False, stop=True), r=[b_w[i], cst], w=[b_py[pi]])
                            P.op("act", lambda eng, tt=tt, hh=hh, pi=pi, y2=y2: eng.activation(
                                out=yo[y2][:, hh * 512:(hh + 1) * 512], in_=py[pi][:, :], func=AF.Copy,
                                scale=xr[j3][:, tt, 1024:ROWW].bitcast(F32)[:, e:e + 1]), r=[b_py[pi], b_xr[j3]], w=[b_yo[y2]])
                        r0 = s0 + tt * 128
                        P.dma("sp", lambda eng, y2=y2, r0=r0: eng.dma_start(out=YS.ap()[r0:r0 + 128, :], in_=yo[y2][:]),
                              b_yo[y2], r=[b_yo[y2]], acc=[YSall])

                load_expert(0)
                if E > 1:
                    load_expert(1)
                nb = len(blocks)
                emit_T(blocks[0])
                emit_GU(blocks[0])
                if nb > 1:
                    emit_T(blocks[1])
                for bi, blk in enumerate(blocks):
                    e, sbk, j, j3 = blk
                    if bi + 1 < nb:
                        emit_GU(blocks[bi + 1])
                    if bi + 2 < nb:
                        emit_T(blocks[bi + 2])
                    emit_D(blk)
                    if sbk == NSB - 1 and e + 2 < E:
                        load_expert(e + 2)
                P.flush()

            with contextlib.ExitStack() as st:
                g1bc = sb("c_g1", [128, D], st=st)
                lng = sb("c_lng", [128, D], st=st)
                lnb = sb("c_lnb", [128, D], st=st)
                b_bc = Buf()
                pbc = ps("c_pbc", [128, 512], st=st)
                bc_from_cols(st, "c_g1", lambda k: ada[:, lj, 16 + k:17 + k], pbc, g1bc, b_bc)
                P.dma("sp", lambda eng: eng.dma_start(out=lng[:], in_=ln_g_bc.ap()[lj]), b_bc, acc=[b_bc])
                P.dma("sp", lambda eng: eng.dma_start(out=lnb[:], in_=ln_b_bc.ap()[lj]), b_bc, acc=[b_bc])
                NB = 2
                xt = [sb("c_x%d" % i, [128, D], st=st) for i in range(NB)]
                yg = [sb("c_yg%d" % i, [128, 4, D], st=st) for i in range(NB)]
                t0 = [sb("c_t0%d" % i, [128, D], st=st) for i in range(NB)]
                t1 = [sb("c_t1%d" % i, [128, D], st=st) for i in range(NB)]
                xo = [sb("c_xo%d" % i, [128, D], st=st) for i in range(NB)]
                small = [(sb("c_st%d" % i, [128, 2, 6], st=st), sb("c_mv%d" % i, [128, 2], st=st),
                          sb("c_rs%d" % i, [128, 1], st=st)) for i in range(NB)]
                b_xt = [Buf() for _ in range(NB)]
                b_yg = [Buf() for _ in range(NB)]
                b_t0 = [Buf() for _ in range(NB)]
                b_t1 = [Buf() for _ in range(NB)]
                b_xo = [Buf() for _ in range(NB)]
                for t in range(NT):
                    i = t % NB
                    load_x(src_kind, t, xt[i][:], b_xt[i])
                    for kk in range(TOPK):
                        P.dma("pool", lambda eng, t=t, kk=kk, i=i: eng.indirect_dma_start(
                            out=yg[i][:, kk, :], out_offset=None, in_=YS.ap(),
                            in_offset=bass.IndirectOffsetOnAxis(ap=sidx[:, t, kk:kk + 1], axis=0)),
                            b_yg[i], r=[YSall], acc=[b_yg[i]])
                    P.op("dve", lambda eng, i=i: eng.tensor_tensor(out=yg[i][:, 0, :], in0=yg[i][:, 0, :], in1=yg[i][:, 1, :],
                                                                 op=ALU.add), r=[b_yg[i]], w=[b_yg[i]])
                    P.op("pool", lambda eng, i=i: eng.tensor_tensor(out=yg[i][:, 2, :], in0=yg[i][:, 2, :], in1=yg[i][:, 3, :],
                                                                  op=ALU.add), r=[b_yg[i]], w=[b_yg[i]])
                    P.op("dve", lambda eng, i=i: eng.tensor_tensor(out=yg[i][:, 0, :], in0=yg[i][:, 0, :], in1=yg[i][:, 2, :],
                                                                 op=ALU.add), r=[b_yg[i]], w=[b_yg[i]])
                    residual_ln(xt[i][:], [(yg[i][:, 0, :], 0, D)], g1bc, lng, lnb, t0[i], t1[i], xo[i][:],
                                b_xt[i], b_yg[i], b_bc, b_t0[i], b_t1[i], b_xo[i], small[i])
                    P.dma("sp", lambda eng, t=t, i=i: eng.dma_start(out=xtile_dram(dst_kind, t), in_=xo[i][:]),
                          b_xo[i], r=[b_xo[i]])
                P.flush()

        def conv_layer(l, src_kind):
            lj = 2 * l
            with contextlib.ExitStack() as st:
                win = sb("a_win", [128, NCH, 3 * D], BF16, st=st)
                wout = sb("a_wout", [128, NCH, D], BF16, st=st)
                b_w = Buf()
                load_w_bf16(win[:], conv_w_in.ap()[l], b_w)
                P.dma("pool", lambda eng: eng.dma_start(out=wout[:], in_=conv_w_out.ap()[l].rearrange("(k p) n -> p k n", p=128)),
                      b_w, acc=[b_w])
                g1bc = sb("a_g1", [128, D], st=st)
                lng = sb("a_lng", [128, D], st=st)
                lnb = sb("a_lnb", [128, D], st=st)
                b_bc = Buf()
                pbc = ps("a_pbc", [128, 512], st=st)
                bc_from_cols(st, "a_g1", lambda k: ada[:, lj, 16 + k:17 + k], pbc, g1bc, b_bc)
                P.dma("sp", lambda eng: eng.dma_start(out=lng[:], in_=ln_g_bc.ap()[lj]), b_bc, acc=[b_bc])
                P.dma("sp", lambda eng: eng.dma_start(out=lnb[:], in_=ln_b_bc.ap()[lj]), b_bc, acc=[b_bc])
                xt = [sb("a_x%d" % i, [128, 4, D], st=st) for i in range(2)]
                b_xt = [Buf() for _ in range(2)]
                hT = [sb("a_hT%d" % i, [128, NCH, 512], BF16, st=st) for i in range(2)]
                b_hT = [Buf() for _ in range(2)]
                z = sb("a_z", [128, NCH, 514], st=st)
                b_z = [Buf() for _ in range(NCH)]
                gcs = [sb("a_gc%d" % i, [128, 512], st=st) for i in range(2)]
                b_gcs = [Buf() for _ in range(2)]
                zc = [sb("a_zc%d" % i, [128, 512], st=st) for i in range(2)]
                b_zc = [Buf() for _ in range(2)]
                vT = [sb("a_vT%d" % i, [128, NCH, 512], BF16, st=st) for i in range(2)]
                b_vT = [Buf() for _ in range(2)]
                t0 = [sb("a_t0%d" % i, [128, D], st=st) for i in range(2)]
                t1 = [sb("a_t1%d" % i, [128, D], st=st) for i in range(2)]
                xo = [sb("a_xo%d" % i, [128, D], st=st) for i in range(2)]
                small = [(sb("a_st%d" % i, [128, 2, 6], st=st), sb("a_mv%d" % i, [128, 2], st=st),
                          sb("a_rs%d" % i, [128, 1], st=st)) for i in range(2)]
                b_t0 = [Buf() for _ in range(2)]
                b_t1 = [Buf() for _ in range(2)]
                b_xo = [Buf() for _ in range(2)]
                ptr = [ps("a_ptr%d" % i, [128, 512], st=st) for i in range(2)]
                b_ptr = [Buf() for _ in range(2)]
                pc = ps("a_pc", [128, 512], st=st)
                pb_ = ps("a_pb", [128, 512], st=st)
                pu = ps("a_pu", [128, 512], st=st)
                b_pc, b_pb, b_pu = Buf(), Buf(), Buf()
                po = ps("a_po", [128, 1024], st=st)
                b_po = Buf()
                P.op("dve", lambda eng: eng.memset(z[:], 0.0), w=b_z)
                NG = T1 // 512
                tcnt = 0
                for g in range(NG):
                    gi = g % 2
                    for tt in range(4):
                        t = g * 4 + tt
                        P.dma("sp", lambda eng, gi=gi, tt=tt, t=t: eng.dma_start(out=xt[gi][:, tt, :], in_=xtile_dram(src_kind, t)),
                              b_xt[gi], acc=[b_xt[gi]], w=[])
                    for k in range(NCH):
                        pi = k % 2
                        for tt in range(4):
                            P.op("pe", lambda eng, k=k, tt=tt, gi=gi, pi=pi: eng.transpose(
                                ptr[pi][:, tt * 128:(tt + 1) * 128], xt[gi][:, tt, k * 128:(k + 1) * 128], ident_f[:]),
                                r=[b_xt[gi], cst], w=[b_ptr[pi]])
                        P.op("act", lambda eng, k=k, gi=gi, pi=pi: eng.activation(
                            out=hT[gi][:, k, :], in_=ptr[pi][:], func=AF.Identity, bias=ada[:, lj, k:k + 1],
                            scale=ada[:, lj, 8 + k:9 + k]), r=[b_ptr[pi]], w=[b_hT[gi]])
                    for f in range(NCH):
                        q2 = f % 2
                        for (pp, bb, off) in [(pc, b_pc, 0), (pu, b_pu, 2 * D), (pb_, b_pb, D)]:
                            for k in range(NCH):
                                P.op("pe", lambda eng, pp=pp, off=off, f=f, k=k, gi=gi: eng.matmul(
                                    pp[:], lhsT=win[:, k, off + f * 128:off + (f + 1) * 128], rhs=hT[gi][:, k, :],
                                    start=(k == 0), stop=(k == NCH - 1)), r=[b_w, b_hT[gi]], w=[bb])
                        P.op("act", lambda eng, q2=q2: eng.activation(out=gcs[q2][:], in_=pc[:], func=AF.Copy),
                             r=[b_pc], w=[b_gcs[q2]])
                        P.op("dve", lambda eng, f=f, q2=q2: eng.tensor_tensor(out=z[:, f, 2:514], in0=pu[:], in1=gcs[q2][:],
                                                                            op=ALU.mult), r=[b_pu, b_gcs[q2]], w=[b_z[f]])
                        P.op("pool", lambda eng, f=f, q2=q2: eng.tensor_scalar(
                            out=zc[q2][:], in0=z[:, f, 2:514], scalar1=convw[:, l, f, 2:3], scalar2=None, op0=ALU.mult),
                            r=[b_z[f], cst], w=[b_zc[q2]])
                        P.op("dve", lambda eng, f=f, q2=q2: eng.scalar_tensor_tensor(
                            out=zc[q2][:], in0=z[:, f, 1:513], scalar=convw[:, l, f, 1:2], in1=zc[q2][:], op0=ALU.mult,
                            op1=ALU.add), r=[b_z[f], b_zc[q2], cst], w=[b_zc[q2]])
                        P.op("dve", lambda eng, f=f, q2=q2: eng.scalar_tensor_tensor(
                            out=zc[q2][:], in0=z[:, f, 0:512], scalar=convw[:, l, f, 0:1], in1=zc[q2][:], op0=ALU.mult,
                            op1=ALU.add), r=[b_z[f], b_zc[q2], cst], w=[b_zc[q2]])
                        P.op("dve", lambda eng, f=f, q2=q2, gi=gi: eng.tensor_tensor(
                            out=vT[gi][:, f, :], in0=pb_[:], in1=zc[q2][:], op=ALU.mult), r=[b_pb, b_zc[q2]], w=[b_vT[gi]])
                        P.op("pool", lambda eng, f=f: eng.tensor_copy(out=z[:, f, 0:2], in_=z[:, f, 512:514]),
                             r=[b_z[f]], w=[b_z[f]])
                    for tt in range(4):
                        t = g * 4 + tt
                        i = tcnt % 2
                        tcnt += 1
                        for hh in range(2):
                            for f in range(NCH):
                                P.op("pe", lambda eng, tt=tt, hh=hh, f=f, gi=gi: eng.matmul(
                                    po[:, hh * 512:(hh + 1) * 512], lhsT=vT[gi][:, f, tt * 128:(tt + 1) * 128],
                                    rhs=wout[:, f, hh * 512:(hh + 1) * 512], start=(f == 0), stop=(f == NCH - 1)),
                                    r=[b_vT[gi], b_w], w=[b_po])
                        residual_ln(xt[gi][:, tt, :], [(po[:, 0:512], 0, 512), (po[:, 512:1024], 512, 512)], g1bc, lng, lnb,
                                    t0[i], t1[i], xo[i][:], b_xt[gi], b_po, b_bc, b_t0[i], b_t1[i], b_xo[i], small[i])
                        P.dma("sp", lambda eng, t=t, i=i: eng.dma_start(out=xtile_dram("XA", t), in_=xo[i][:]),
                              b_xo[i], r=[b_xo[i]])
                P.flush()

        def kv_stage():
            with contextlib.ExitStack() as st:
                wk = sb("k_w", [128, NCH, 2 * D + H], BF16, st=st)
                b_w = Buf()
                load_w_bf16(wk[:], w_kvf.ap(), b_w)
                zt = sb("k_zt", [128, T2], BF16, st=st)
                zf = sb("k_zf", [128, 512], st=st)
                b_z = Buf()
                P.op("dve", lambda eng: eng.memset(zt[:], 0.0), w=[b_z])
                P.op("pool", lambda eng: eng.memset(zf[:], 0.0), w=[b_z])
                for h in range(H):
                    P.dma("sp", lambda eng, h=h: eng.dma_start(out=KT[h].ap()[128:192, :], in_=zt[0:64, :]), b_z, r=[b_z])
                    P.dma("sp", lambda eng, h=h: eng.dma_start(out=VA[h].ap()[256:384, :], in_=zt[:, :]), b_z, r=[b_z])
                P.dma("sp", lambda eng: eng.dma_start(out=LF.ap()[256:384, :], in_=zf[:, 0:NT2 * H]), b_z, r=[b_z])
                for cc in range(T2 // 512):
                    P.dma("sp", lambda eng, cc=cc: eng.dma_start(out=LFT.ap()[32:48, cc * 512:(cc + 1) * 512], in_=zf[0:16, :]),
                          b_z, r=[b_z])
                xt = [sb("k_x%d" % i, [128, 4, D], st=st) for i in range(2)]
                b_xt = [Buf() for _ in range(2)]
                hT = [sb("k_hT%d" % i, [128, NCH, 512], BF16, st=st) for i in range(2)]
                b_hT = [Buf() for _ in range(2)]
                kst = [sb("k_kst%d" % i, [64, H, 512], BF16, st=st) for i in range(2)]
                b_kst = [Buf() for _ in range(2)]
                vst = [sb("k_vst%d" % i, [128, H, 128], BF16, st=st) for i in range(2)]
                b_vst = [Buf() for _ in range(2)]
                for i in range(2):
                    P.op("dve", lambda eng, i=i: eng.memset(vst[i][:], 1.0), w=[b_vst[i]])
                lfall = sb("k_lfall", [128, NT1, H], st=st)
                lftg = [sb("k_lftg%d" % i, [16, 512], st=st) for i in range(2)]
                b_lfall = Buf()
                b_lftg = [Buf() for _ in range(2)]
                zl = sb("k_zl", [128, H], st=st)
                l1 = sb("k_l1", [128, H], st=st)
                lsum = sb("k_lsum", [128, H], st=st)
                lfc = sb("k_lfc", [128, H], st=st)
                b_s, b_lsum = Buf(), Buf()
                P.op("dve", lambda eng: eng.memset(lsum[:], 0.0), w=[b_lsum])
                ptr = [ps("k_ptr%d" % i, [128, 512], st=st) for i in range(2)]
                b_ptr = [Buf() for _ in range(2)]
                pk = [ps("k_pk%d" % i, [128, 512], st=st) for i in range(2)]
                b_pk = [Buf() for _ in range(2)]
                pv = ps("k_pv", [128, 1024], st=st)
                b_pv = Buf()
                pf = ps("k_pf", [128, 512], st=st)
                b_pf = Buf()
                vcnt = 0
                for g in range(T1 // 512):
                    gi = g % 2
                    B, gl = g // NGB, g % NGB
                    for tt in range(4):
                        t = g * 4 + tt
                        P.dma("sp", lambda eng, gi=gi, tt=tt, t=t: eng.dma_start(out=xt[gi][:, tt, :], in_=xtile_dram("X1B", t)),
                              b_xt[gi], acc=[b_xt[gi]])
                    for k in range(NCH):
                        pi = k % 2
                        for tt in range(4):
                            P.op("pe", lambda eng, k=k, tt=tt, gi=gi, pi=pi: eng.transpose(
                                ptr[pi][:, tt * 128:(tt + 1) * 128], xt[gi][:, tt, k * 128:(k + 1) * 128], ident_f[:]),
                                r=[b_xt[gi], cst], w=[b_ptr[pi]])
                        P.op("act", lambda eng, k=k, gi=gi, pi=pi: eng.activation(
                            out=hT[gi][:, k, :], in_=ptr[pi][:], func=AF.Identity, bias=kvada[:, k:k + 1],
                            scale=kvada[:, 8 + k:9 + k]), r=[b_ptr[pi]], w=[b_hT[gi]])
                    for h in range(H):
                        q2 = h % 2
                        for k in range(NCH):
                            P.op("pe", lambda eng, h=h, k=k, q2=q2, gi=gi: eng.matmul(
                                pk[q2][0:64, :], lhsT=wk[:, k, h * 64:(h + 1) * 64], rhs=hT[gi][:, k, :],
                                start=(k == 0), stop=(k == NCH - 1)), r=[b_w, b_hT[gi]], w=[b_pk[q2]])
                        P.op("act", lambda eng, h=h, q2=q2, gi=gi: eng.activation(out=kst[gi][:, h, :], in_=pk[q2][0:64, :],
                                                                                func=AF.Copy), r=[b_pk[q2]], w=[b_kst[gi]])
                    for h in range(H):
                        P.dma("sp", lambda eng, h=h, gi=gi, B=B, gl=gl: eng.dma_start(
                            out=KT[h].ap()[B * 64:(B + 1) * 64, gl * 512:(gl + 1) * 512], in_=kst[gi][:, h, :]),
                            b_kst[gi], r=[b_kst[gi]])
                    for tt in range(4):
                        t = g * 4 + tt
                        j = gl * 4 + tt
                        vi = vcnt % 2
                        vcnt += 1
                        for hh in range(2):
                            for k in range(NCH):
                                P.op("pe", lambda eng, tt=tt, hh=hh, k=k, gi=gi: eng.matmul(
                                    pv[:, hh * 512:(hh + 1) * 512], lhsT=hT[gi][:, k, tt * 128:(tt + 1) * 128],
                                    rhs=wk[:, k, D + hh * 512:D + (hh + 1) * 512], start=(k == 0), stop=(k == NCH - 1)),
                                    r=[b_w, b_hT[gi]], w=[b_pv])
                        P.op("act", lambda eng, vi=vi: eng.activation(out=vst[vi][:, :, 0:64],
                                                                     in_=pv[:].rearrange("p (h d) -> p h d", d=64),
                                                                     func=AF.Copy), r=[b_pv], w=[b_vst[vi]])
                        for h in range(H):
                            P.dma("sp", lambda eng, h=h, vi=vi, B=B, j=j: eng.dma_start(
                                out=VA[h].ap()[B * 128:(B + 1) * 128, j * 128:(j + 1) * 128], in_=vst[vi][:, h, :]),
                                b_vst[vi], r=[b_vst[vi]])
                        for k in range(NCH):
                            P.op("pe", lambda eng, tt=tt, k=k, gi=gi: eng.matmul(
                                pf[:, 0:H], lhsT=hT[gi][:, k, tt * 128:(tt + 1) * 128], rhs=wk[:, k, 2 * D:2 * D + H],
                                start=(k == 0), stop=(k == NCH - 1)), r=[b_w, b_hT[gi]], w=[b_pf])
                        P.op("dve", lambda eng: eng.tensor_tensor(out=zl[:], in0=pf[:, 0:H], in1=bfb[:], op=ALU.add),
                             r=[b_pf, cst], w=[b_s])
                        P.op("act", lambda eng: eng.activation(out=zl[:], in_=zl[:], func=AF.Exp, scale=-1.0), r=[b_s], w=[b_s])
                        P.op("act", lambda eng: eng.activation(out=l1[:], in_=zl[:], func=AF.Ln, bias=1.0, scale=1.0),
                             r=[b_s], w=[b_s])
                        P.op("pe", lambda eng: eng.matmul(pf[:, 64:64 + H], lhsT=uincl[:], rhs=l1[:], start=True, stop=False),
                             r=[b_s, cst], w=[b_pf])
                        P.op("pe", lambda eng: eng.matmul(pf[:, 64:64 + H], lhsT=ones_f[:], rhs=lsum[:], start=False, stop=True),
                             r=[b_lsum, cst], w=[b_pf])
                        P.op("dve", lambda eng: eng.tensor_tensor(out=lsum[:], in0=lsum[:], in1=l1[:], op=ALU.add),
                             r=[b_s, b_lsum], w=[b_lsum])
                        P.op("dve", lambda eng, t=t: eng.tensor_scalar(out=lfall[:, t, :], in0=pf[:, 64:64 + H], scalar1=-1.0,
                                                                     scalar2=None, op0=ALU.mult), r=[b_pf], acc=[b_lfall])
                        P.op("pe", lambda eng, t=t: eng.transpose(pf[0:16, 128:256], lfall[:, t, :], ident_f[:]),
                             r=[b_lfall, cst], w=[b_pf])
                        P.op("act", lambda eng, tt=tt, gi=gi: eng.activation(out=lftg[gi][:, tt * 128:(tt + 1) * 128],
                                                                            in_=pf[0:16, 128:256], func=AF.Copy),
                             r=[b_pf], w=[b_lftg[gi]])
                    P.dma("sp", lambda eng, gi=gi, B=B, gl=gl: eng.dma_start(
                        out=LFT.ap()[B * 16:(B + 1) * 16, gl * 512:(gl + 1) * 512], in_=lftg[gi][:]), b_lftg[gi], r=[b_lftg[gi]])
                for B in range(2):
                    P.dma("sp", lambda eng, B=B: eng.dma_start(
                        out=LF.ap()[B * 128:(B + 1) * 128, :], in_=lfall[:, B * NT2:(B + 1) * NT2, :].rearrange("p a b -> p (a b)")),
                        b_lfall, r=[b_lfall])
                P.flush()

        def load_own_group(l, g, tile4, b, bufacc):
            if l == 2:
                P.dma("pool", lambda eng: eng.indirect_dma_start(
                    out=tile4[:].rearrange("p a b -> p (a b)"), out_offset=None, in_=X1B[g].ap(),
                    in_offset=bass.IndirectOffsetOnAxis(ap=idxs[:, 0:1], axis=0)), b, r=[cst], w=[b])
            else:
                for tt in range(4):
                    P.dma("sp", lambda eng, tt=tt: eng.dma_start(out=tile4[:, tt, :], in_=xtile_dram("XB", g * 4 + tt)),
                          b, acc=[b])

        def attn_layer(l):
            lj = 2 * l
            ja = l - 2
            with contextlib.ExitStack() as st:
                wq = sb("q_w", [128, NCH, D], BF16, st=st)
                b_w = Buf()
                load_w_bf16(wq[:], attn_w_q.ap()[ja], b_w)
                lft = sb("q_lft", [16, T2], st=st)
                b_lft = Buf()
                P.dma("pool", lambda eng: eng.indirect_dma_start(
                    out=lft[:], out_offset=None, in_=LFT.ap(), in_offset=bass.IndirectOffsetOnAxis(ap=idx16[0:16, 0:1], axis=0)),
                    b_lft, r=[cst], w=[b_lft])
                xt = [sb("q_x%d" % i, [128, 4, D], st=st) for i in range(2)]
                b_xt = [Buf() for _ in range(2)]
                hT = [sb("q_hT%d" % i, [128, NCH, 512], BF16, st=st) for i in range(2)]
                b_hT = [Buf() for _ in range(2)]
                qa = [sb("q_qa%d" % i, [128, 512], BF16, st=st) for i in range(3)]
                b_qa = [Buf() for _ in range(3)]
                t96 = sb("q_t96", [128, 512], BF16, st=st)
                b_t96 = Buf()
                for i in range(3):
                    P.op("dve", lambda eng, i=i: eng.memset(qa[i][:], 0.0), w=[b_qa[i]])
                ptr = [ps("q_ptr%d" % i, [128, 512], st=st) for i in range(2)]
                b_ptr = [Buf() for _ in range(2)]
                pq = [ps("q_pq%d" % i, [128, 512], st=st) for i in range(2)]
                b_pq = [Buf() for _ in range(2)]
                pa = [ps("q_pa%d" % i, [128, 512], st=st) for i in range(2)]
                b_pa = [Buf() for _ in range(2)]
                qc = 0
                for g in range(NGB):
                    gi = g % 2
                    load_own_group(l, g, xt[gi], b_xt[gi], None)
                    for k in range(NCH):
                        pi = k % 2
                        for tt in range(4):
                            P.op("pe", lambda eng, k=k, tt=tt, gi=gi, pi=pi: eng.transpose(
                                ptr[pi][:, tt * 128:(tt + 1) * 128], xt[gi][:, tt, k * 128:(k + 1) * 128], ident_f[:]),
                                r=[b_xt[gi], cst], w=[b_ptr[pi]])
                        P.op("act", lambda eng, k=k, gi=gi, pi=pi: eng.activation(
                            out=hT[gi][:, k, :], in_=ptr[pi][:], func=AF.Identity, bias=ada[:, lj, k:k + 1],
                            scale=ada[:, lj, 8 + k:9 + k]), r=[b_ptr[pi]], w=[b_hT[gi]])
                    for h in range(H):
                        q2 = h % 2
                        qi = qc % 3
                        qc += 1
                        for k in range(NCH):
                            P.op("pe", lambda eng, h=h, k=k, q2=q2, gi=gi: eng.matmul(
                                pq[q2][0:64, :], lhsT=wq[:, k, h * 64:(h + 1) * 64], rhs=hT[gi][:, k, :],
                                start=(k == 0), stop=(k == NCH - 1)), r=[b_w, b_hT[gi]], w=[b_pq[q2]])
                        P.op("pe", lambda eng, h=h, q2=q2, g=g: eng.matmul(
                            pa[q2][:, :], lhsT=selh[:, h, :], rhs=lft[:, g * 512:(g + 1) * 512], start=True, stop=True),
                            r=[b_lft, cst], w=[b_pa[q2]])
                        P.op("act", lambda eng, q2=q2, qi=qi: eng.activation(out=qa[qi][0:64, :], in_=pq[q2][0:64, :], func=AF.Copy),
                             r=[b_pq[q2]], w=[b_qa[qi]])
                        P.op("act", lambda eng, q2=q2, qi=qi: eng.activation(out=qa[qi][64:65, :], in_=pa[q2][64:65, :],
                                                                            func=AF.Copy, scale=1.0 / SCALE),
                             r=[b_pa[q2]], w=[b_qa[qi]])
                        P.op("act", lambda eng, q2=q2: eng.activation(out=t96[96:97, :], in_=pa[q2][96:97, :], func=AF.Copy,
                                                                     scale=1.0 / SCALE), r=[b_pa[q2]], w=[b_t96])
                        P.op("dve", lambda eng, q2=q2, qi=qi: eng.scalar_tensor_tensor(
                            out=qa[qi][96:97, :], in0=pa[q2][96:97, :], scalar=1.0 / SCALE, in1=t96[96:97, :], op0=ALU.mult,
                            op1=ALU.subtract), r=[b_pa[q2], b_t96], w=[b_qa[qi]])
                        P.dma("sp", lambda eng, h=h, g=g, qi=qi: eng.dma_start(out=QT.ap()[h, :, g * 512:(g + 1) * 512],
                                                                              in_=qa[qi][:, :]), b_qa[qi], r=[b_qa[qi]])
                P.flush()

            with contextlib.ExitStack() as st:
                kt = [sb("t_kt%d" % i, [128, 2 * T2], BF16, st=st) for i in range(2)]
                va = [sb("t_va%d" % i, [128, 2 * NT2, 128], BF16, st=st) for i in range(2)]
                qt = [sb("t_qt%d" % i, [128, T2], BF16, st=st) for i in range(2)]
                b_kt = [Buf() for _ in range(2)]
                b_va = [Buf() for _ in range(2)]
                b_qt = [Buf() for _ in range(2)]
                b_aug = [Buf() for _ in range(2)]
                for i in range(2):
                    P.op("pool", lambda eng, i=i: eng.memset(kt[i][64:128, :], 0.0), w=[b_aug[i]])
                    P.op("pool", lambda eng, i=i: eng.memset(kt[i][64:65, :], 1.0), w=[b_aug[i]])
                    P.op("pool", lambda eng, i=i: eng.memset(kt[i][96:97, :], 1.0), w=[b_aug[i]])
                nlf = sb("t_nlf", [128, 2, NT2, H], st=st)
                b_nlf = Buf()
                for blk, col in [(0, 1), (1, 0)]:
                    P.dma("pool", lambda eng, blk=blk, col=col: eng.indirect_dma_start(
                        out=nlf[:, blk, :, :].rearrange("p a b -> p (a b)"), out_offset=None, in_=LF.ap(),
                        in_offset=bass.IndirectOffsetOnAxis(ap=idxs[:, col:col + 1], axis=0)), b_nlf, r=[cst], acc=[b_nlf])
                P.op("dve", lambda eng: eng.tensor_scalar(out=nlf[:].rearrange("p a b c -> p (a b c)"),
                                                        in0=nlf[:].rearrange("p a b c -> p (a b c)"), scalar1=-1.0,
                                                        scalar2=None, op0=ALU.mult), r=[b_nlf], w=[b_nlf])
                pt = [sb("t_pt%d" % i, [128, 512], BF16, st=st) for i in range(3)]
                b_pt = [Buf() for _ in range(3)]
                srow = sb("t_srow", [128, 512], st=st)
                b_srow = Buf()
                P.op("dve", lambda eng: eng.memset(srow[:], 0.0), w=[b_srow])
                rec = sb("t_rec", [64, 512], st=st)
                b_rec = Buf()
                on = [sb("t_on%d" % i, [64, 512], BF16, st=st) for i in range(2)]
                b_on = [Buf() for _ in range(2)]
                pss = [ps("t_ps%d" % i, [128, 512], st=st) for i in range(3)]
                b_ps = [Buf() for _ in range(3)]
                po = [ps("t_po%d" % i, [128, 512], st=st) for i in range(2)]
                b_po = [Buf() for _ in range(2)]
                pr = ps("t_pr", [128, 512], st=st)
                b_pr = Buf()

                def load_head(h):
                    i = h % 2
                    P.dma("pool", lambda eng: eng.indirect_dma_start(
                        out=kt[i][0:64, 0:T2], out_offset=None, in_=KT[h].ap(),
                        in_offset=bass.IndirectOffsetOnAxis(ap=idxs[0:64, 3:4], axis=0)), b_kt[i], r=[cst], acc=[b_kt[i]])
                    P.dma("pool", lambda eng: eng.indirect_dma_start(
                        out=kt[i][0:64, T2:2 * T2], out_offset=None, in_=KT[h].ap(),
                        in_offset=bass.IndirectOffsetOnAxis(ap=idxs[0:64, 2:3], axis=0)), b_kt[i], r=[cst], acc=[b_kt[i]])
                    P.dma("pool", lambda eng: eng.indirect_dma_start(
                        out=va[i][:, 0:NT2, :].rearrange("p a b -> p (a b)"), out_offset=None, in_=VA[h].ap(),
                        in_offset=bass.IndirectOffsetOnAxis(ap=idxs[:, 1:2], axis=0)), b_va[i], r=[cst], acc=[b_va[i]])
                    P.dma("pool", lambda eng: eng.indirect_dma_start(
                        out=va[i][:, NT2:2 * NT2, :].rearrange("p a b -> p (a b)"), out_offset=None, in_=VA[h].ap(),
                        in_offset=bass.IndirectOffsetOnAxis(ap=idxs[:, 0:1], axis=0)), b_va[i], r=[cst], acc=[b_va[i]])
                    P.dma("sp", lambda eng: eng.dma_start(out=qt[i][:, :], in_=QT.ap()[h]), b_qt[i], w=[b_qt[i]])

                load_head(0)
                sc = 0
                gc = 0
                for h in range(H):
                    i = h % 2
                    if h + 1 < H:
                        load_head(h + 1)
                    for g in range(NGB):
                        oi = gc % 2
                        gc += 1
                        steps = [(0, jt) for jt in range(NT2)] + [(1, jt) for jt in range(4 * g + 4)]
                        nsteps = len(steps)
                        sis = []

                        def emit_qk(s_, blk, jt, si):
                            diag = (blk == 1 and jt >= 4 * g)
                            c0 = (jt - 4 * g) * 128 if diag else 0
                            koff = blk * T2 + jt * 128
                            P.op("pe", lambda eng, si=si, c0=c0, koff=koff, g=g, i=i, diag=diag: eng.matmul(
                                pss[si][:, c0:512], lhsT=kt[i][:, koff:koff + 128], rhs=qt[i][:, g * 512 + c0:(g + 1) * 512],
                                start=True, stop=(not diag)), r=[b_kt[i], b_aug[i], b_qt[i]], w=[b_ps[si]])
                            if diag:
                                P.op("pe", lambda eng, si=si, c0=c0: eng.matmul(
                                    pss[si][:, c0:c0 + 128], lhsT=ident_b[:], rhs=maskb[:], start=False, stop=True),
                                    r=[cst], w=[b_ps[si]])
                            P.op("act", lambda eng, si=si, c0=c0, blk=blk, jt=jt, h=h: eng.activation(
                                out=pt[si][:, c0:512], in_=pss[si][:, c0:512], func=AF.Exp, bias=nlf[:, blk, jt, h:h + 1],
                                scale=SCALE), r=[b_ps[si], b_nlf], w=[b_pt[si]])
                            return c0

                        def emit_pv(s_, blk, jt, si, c0):
                            P.op("pe", lambda eng, si=si, c0=c0, blk=blk, jt=jt, oi=oi, i=i, s_=s_, last=(s_ == nsteps - 1): eng.matmul(
                                po[oi][:, c0:512], lhsT=va[i][:, blk * NT2 + jt, :], rhs=pt[si][:, c0:512],
                                start=(s_ == 0), stop=last), r=[b_va[i], b_pt[si]], w=[b_po[oi]])

                        pend = None
                        for s_, (blk, jt) in enumerate(steps):
                            si = sc % 3
                            sc += 1
                            c0 = emit_qk(s_, blk, jt, si)
                            if pend is not None:
                                emit_pv(*pend)
                            pend = (s_, blk, jt, si, c0)
                        emit_pv(*pend)
                        P.op("act", lambda eng, oi=oi: eng.activation(out=srow[64:128, :], in_=po[oi][64:128, :], func=AF.Copy),
                             r=[b_po[oi]], w=[b_srow])
                        P.op("pe", lambda eng: eng.matmul(pr[0:64, :], lhsT=sel64[:], rhs=srow[:], start=True, stop=True),
                             r=[b_srow, cst], w=[b_pr])
                        P.op("dve", lambda eng: eng.reciprocal(out=rec[:], in_=pr[0:64, :]), r=[b_pr], w=[b_rec])
                        P.op("dve", lambda eng, oi=oi: eng.tensor_tensor(out=on[oi][:], in0=po[oi][0:64, :], in1=rec[:], op=ALU.mult),
                             r=[b_po[oi], b_rec], w=[b_on[oi]])
                        P.dma("sp", lambda eng, h=h, g=g, oi=oi: eng.dma_start(out=OT.ap()[h, :, g * 512:(g + 1) * 512], in_=on[oi][:]),
                              b_on[oi], r=[b_on[oi]])
                P.flush()

            with contextlib.ExitStack() as st:
                wo = sb("o_w", [64, H, D], BF16, st=st)
                b_w = Buf()
                P.dma("pool", lambda eng: eng.dma_start(out=wo[:], in_=attn_w_o.ap()[ja].rearrange("(h d) n -> d h n", d=64)),
                      b_w, w=[b_w])
                g1bc = sb("o_g1", [128, D], st=st)
                lng = sb("o_lng", [128, D], st=st)
                lnb = sb("o_lnb", [128, D], st=st)
                b_bc = Buf()
                pbc = ps("o_pbc", [128, 512], st=st)
                bc_from_cols(st, "o_g1", lambda k: ada[:, lj, 16 + k:17 + k], pbc, g1bc, b_bc)
                P.dma("sp", lambda eng: eng.dma_start(out=lng[:], in_=ln_g_bc.ap()[lj]), b_bc, acc=[b_bc])
                P.dma("sp", lambda eng: eng.dma_start(out=lnb[:], in_=ln_b_bc.ap()[lj]), b_bc, acc=[b_bc])
                xt = [sb("o_x%d" % i, [128, 4, D], st=st) for i in range(2)]
                b_xt = [Buf() for _ in range(2)]
                ot = [sb("o_ot%d" % i, [64, H, 128], BF16, st=st) for i in range(2)]
                b_ot = [Buf() for _ in range(2)]
                t0 = [sb("o_t0%d" % i, [128, D], st=st) for i in range(2)]
                t1 = [sb("o_t1%d" % i, [128, D], st=st) for i in range(2)]
                xo = [sb("o_xo%d" % i, [128, D], st=st) for i in range(2)]
                small = [(sb("o_st%d" % i, [128, 2, 6], st=st), sb("o_mv%d" % i, [128, 2], st=st),
                          sb("o_rs%d" % i, [128, 1], st=st)) for i in range(2)]
                b_t0 = [Buf() for _ in range(2)]
                b_t1 = [Buf() for _ in range(2)]
                b_xo = [Buf() for _ in range(2)]
                po2 = ps("o_po", [128, 1024], st=st)
                b_po2 = Buf()
                for g in range(NGB):
                    gi = g % 2
                    load_own_group(l, g, xt[gi], b_xt[gi], None)
                    for tt in range(4):
                        t = g * 4 + tt
                        i = t % 2
                        P.dma("sp", lambda eng, t=t, i=i: eng.dma_start(
                            out=ot[i][:], in_=OT.ap()[:, :, t * 128:(t + 1) * 128].rearrange("h d c -> d h c")),
                            b_ot[i], w=[b_ot[i]])
                        for hh in range(2):
                            for h in range(H):
                                P.op("pe", lambda eng, hh=hh, h=h, i=i: eng.matmul(
                                    po2[:, hh * 512:(hh + 1) * 512], lhsT=ot[i][:, h, :], rhs=wo[:, h, hh * 512:(hh + 1) * 512],
                                    start=(h == 0), stop=(h == H - 1)), r=[b_ot[i], b_w], w=[b_po2])
                        residual_ln(xt[gi][:, tt, :], [(po2[:, 0:512], 0, 512), (po2[:, 512:1024], 512, 512)], g1bc, lng, lnb,
                                    t0[i], t1[i], xo[i][:], b_xt[gi], b_po2, b_bc, b_t0[i], b_t1[i], b_xo[i], small[i])
                        P.dma("sp", lambda eng, t=t, i=i: eng.dma_start(out=xtile_dram("XA", t), in_=xo[i][:]),
                              b_xo[i], r=[b_xo[i]])
                P.flush()

        upto = cfg.upto
        conv_layer(0, "x")
        moe(0, T1, cfg.C1, 0, "XA", "XB")
        if upto >= 1:
            conv_layer(1, "XB")
            moe(1, T1, cfg.C1, 0, "XA", "X1B")
            kv_stage()
        if upto >= 2:
            attn_layer(2)
            moe(2, T2, cfg.C2, 1, "XA", "XB")
        if upto >= 3:
            attn_layer(3)
            moe(3, T2, cfg.C2, 1, "XA", "out")
        print("ninstr", P.ninstr, "ndma sems", P.ndma, flush=True)

    return nc, dbg


def _consts(cfg, hf):
    E = cfg.E
    c = {}
    c["ident_f"] = np.eye(128, dtype=np.float32)
    c["ident_b"] = np.eye(128, dtype=np.float32).astype(ml_dtypes.bfloat16)
    iu = np.arange(128)
    c["ustrict"] = (iu[:, None] < iu[None, :]).astype(np.float32)
    c["uincl"] = (iu[:, None] <= iu[None, :]).astype(np.float32)
    c["ones_f"] = np.ones((128, 128), np.float32)
    c["maskb"] = np.where(iu[None, :] >= iu[:, None], 0.0, -30000.0).astype(np.float32).astype(ml_dtypes.bfloat16)
    eb = np.zeros((128, 4, E), np.float32)
    eb[:, 0, :] = np.arange(E)[None, :] * cfg.C1 + 1
    eb[:, 1, :] = np.arange(E)[None, :] * cfg.C2 + 1
    eb[:, 2, :] = (np.arange(E)[None, :] + 1) * cfg.C1
    eb[:, 3, :] = (np.arange(E)[None, :] + 1) * cfg.C2
    c["ebase"] = eb
    selh = np.zeros((16, H, 128), np.float32)
    for h in range(H):
        selh[h, h, 64] = 1.0
        selh[h, h, 96] = 1.0
    c["selh"] = selh
    s64 = np.zeros((128, 64), np.float32)
    s64[64 + np.arange(64), np.arange(64)] = 1.0
    c["sel64"] = s64
    own, prev = (0, 2) if hf == 0 else (1, 0)
    idx = np.zeros((128, 4), np.int32)
    idx[:, 0] = own * 128 + iu
    idx[:, 1] = prev * 128 + iu
    idx[:, 2] = own * 64 + (iu % 64)
    idx[:, 3] = prev * 64 + (iu % 64)
    c["idxs"] = idx
    c["idx16"] = (own * 16 + (iu % 16)).astype(np.int32).reshape(128, 1)
    return c


def _prep_shared(inp, cfg):
    E = cfg.E
    f = lambda a: np.ascontiguousarray(np.asarray(a, dtype=np.float32))
    s = {}
    s["conv_w_in"] = f(inp["conv_w_in"])
    s["conv_wT"] = f(np.asarray(inp["conv_w"]).reshape(2, 3, NCH, 128).transpose(3, 0, 2, 1))
    s["conv_w_out"] = f(inp["conv_w_out"])
    s["kv_ada_w"] = f(inp["kv_ada_w"])
    s["kv_ada_bT"] = f(np.asarray(inp["kv_ada_b"]).reshape(16, 128).T)
    s["w_kvf"] = f(inp["w_kvf"])
    s["b_f_bc"] = f(np.broadcast_to(np.asarray(inp["b_f"])[None, :], (128, H)))
    s["attn_w_q"] = f(inp["attn_w_q"])
    s["attn_w_o"] = f(inp["attn_w_o"])
    s["ada_w"] = f(np.asarray(inp["ada_w"]).reshape(8, D, 3 * D))
    s["ada_bT"] = f(np.asarray(inp["ada_b"]).reshape(8, 24, 128).transpose(2, 0, 1))
    s["ln_g_bc"] = f(np.broadcast_to(np.asarray(inp["ln_g"]).reshape(8, 1, D), (8, 128, D)))
    s["ln_b_bc"] = f(np.broadcast_to(np.asarray(inp["ln_b"]).reshape(8, 1, D), (8, 128, D)))
    s["router_w"] = f(inp["router_w"])
    s["router_b_bc"] = f(np.broadcast_to(np.asarray(inp["router_b"])[:, None, :], (4, 128, E)))
    s["exp_w_gu"] = f(inp["exp_w_gu"])
    s["exp_b_guT"] = f(np.asarray(inp["exp_b_gu"]).reshape(4, E, 16, 128).transpose(0, 3, 1, 2))
    s["exp_w_down"] = f(inp["exp_w_down"])
    s["exp_b_down"] = f(inp["exp_b_down"])
    return s


def run(inputs, cfg, debug=False):
    x = np.asarray(inputs["x"], dtype=np.float32)
    c = np.asarray(inputs["c"], dtype=np.float32)
    Bn = x.shape[0]
    ncores = 2 * Bn
    nc, dbg = build_program(cfg, debug=debug)
    shared = _prep_shared(inputs, cfg)
    in_maps = []
    for core in range(ncores):
        b, hf = core // 2, core % 2
        m = dict(shared)
        m["x"] = np.ascontiguousarray(x[b])
        m["cT"] = np.ascontiguousarray(c[b].reshape(NCH, 128).T)
        m.update(_consts(cfg, hf))
        in_maps.append(m)
    res = run_bass_kernel_spmd(nc, in_maps, core_ids=list(range(ncores)))
    return res, dbg


def kernel(**inputs):
    x = np.asarray(inputs["x"])
    Bn, S, _ = x.shape
    E = np.asarray(inputs["router_w"]).shape[-1]
    cfg = Cfg(S, E)
    res, _ = run(inputs, cfg)
    out = np.zeros((Bn, S, D), np.float32)
    for core in range(2 * Bn):
        b, hf = core // 2, core % 2
        out[b, hf * cfg.T2:(hf + 1) * cfg.T2] = res.results[core]["out"]
    return out
```
